# Optimizing a Trainium2 kernel written in Bass

```python
import math
import jax, jax.numpy as jnp
from jax import lax
import numpy as np

D_MODEL = 1024
BATCH = 2
SEQ = 8192
DEPTH = 1

CTX_LEN = 256
GRID_W = 64
EPS = 1e-6

HG_HEADS = 4
HG_DK = 128
HG_DV = 128
HG_KDIM = HG_HEADS * HG_DK
HG_WIDTH = HG_HEADS * HG_DV
CHUNK = 32

HY_WIDTH = 512
HY_SHORT = 3
HY_EMB = 33
HY_FILTER_HIDDEN = 64
HY_FILTER_SCALE = 0.05
HY_DECAY_TARGET = 1e-2
HY_FAST_PCT = 0.3
HY_SLOW_PCT = 1.5

SPLIT_POINTS = (HG_KDIM, 2 * HG_KDIM, 3 * HG_KDIM, 3 * HG_KDIM + HG_WIDTH,
                3 * HG_KDIM + 2 * HG_WIDTH, 3 * HG_KDIM + 2 * HG_WIDTH + 3 * HY_WIDTH)
IN_WIDTH = 3 * HG_KDIM + 2 * HG_WIDTH + 3 * HY_WIDTH + 2 * D_MODEL

N_GROUPS = 4
EXPERTS_PER_GROUP = 8
TOP_K = 2
D_EXPERT = 512

kernel_name = "hybrid_hgrn2_hyena_hmoe_dit_block"

F32 = jnp.float32


def rms_norm(x, g):
    xf = x.astype(F32)
    y = xf * lax.rsqrt(jnp.mean(xf * xf, axis=-1, keepdims=True) + EPS)
    return (y * g.astype(F32)).astype(x.dtype)


def adaln(cvec, w, b):
    m = jax.nn.silu(cvec) @ w + b
    return [t[..., None, :] for t in jnp.split(m, 6, axis=-1)]


def modulate(h, shift, scale):
    return h * (1.0 + scale) + shift


def sincos_2d(n_tokens):
    rows = n_tokens // GRID_W
    r = jnp.repeat(jnp.arange(rows, dtype=F32), GRID_W)
    col = jnp.tile(jnp.arange(GRID_W, dtype=F32), rows)
    quarter = D_MODEL // 4
    omega = 1.0 / (10000.0 ** (jnp.arange(quarter, dtype=F32) / quarter))

    def axis_emb(pos):
        a = pos[:, None] * omega[None, :]
        return jnp.concatenate([jnp.sin(a), jnp.cos(a)], axis=-1)

    return jnp.concatenate([axis_emb(r), axis_emb(col)], axis=-1)


def lower_bounds(lb_param):
    p = jax.nn.softmax(lb_param.astype(F32), axis=0)
    return jnp.cumsum(p, axis=0)[:DEPTH]


def heads(t, d):
    B, L, _ = t.shape
    return t.reshape(B, L, HG_HEADS, d).transpose(0, 2, 1, 3)


def flip(t):
    return jnp.flip(t, axis=2)


def hgrn2_keys(z, lb):
    z = z.astype(F32)
    log_f = jnp.log(lb + (1.0 - lb) * jax.nn.sigmoid(z))
    k = (1.0 - lb) * jax.nn.sigmoid(-z)
    return heads(k, HG_DK), heads(log_f, HG_DK)


def gla_chunk_scan(q, k, v, log_f, s0):
    B, H, L, DK = q.shape
    DV = v.shape[-1]
    n = L // CHUNK

    def to_chunks(t):
        return t.reshape(B, H, n, CHUNK, t.shape[-1]).transpose(2, 0, 1, 3, 4)

    b = to_chunks(lax.cumsum(log_f.reshape(B, H, n, CHUNK, DK), axis=3).reshape(B, H, L, DK))
    mask = jnp.tril(jnp.ones((CHUNK, CHUNK), dtype=bool))
    mid = CHUNK // 2

    def step(S, xs):
        qc, kc, vc, bc = xs
        total = bc[:, :, -1:, :]
        ref = bc[:, :, mid:mid + 1, :]
        scores = jnp.einsum('bhtk,bhsk->bhts', qc * jnp.exp(bc - ref), kc * jnp.exp(ref - bc))
        scores = jnp.where(mask, scores, 0.0)
        o = (jnp.einsum('bhtk,bhkv->bhtv', qc * jnp.exp(bc), S)
             + jnp.einsum('bhts,bhsv->bhtv', scores, vc))
        S_new = (jnp.exp(total[:, :, 0, :])[..., None] * S
                 + jnp.einsum('bhsk,bhsv->bhkv', kc * jnp.exp(total - bc), vc))
        return S_new, o

    s_fin, o = lax.scan(step, s0, (to_chunks(q), to_chunks(k), to_chunks(v), b))
    return o.transpose(1, 2, 0, 3, 4).reshape(B, H, L, DV), s_fin


def gla_final_state(k, v, log_f):
    decay = jnp.exp(lax.cumsum(log_f, axis=2, reverse=True) - log_f)
    return jnp.einsum('bhlk,bhlv->bhkv', k * decay, v)


def hgrn2_bidir(q, v, fwd, bwd, s0_f, s0_b):
    o_f, s_f = gla_chunk_scan(q, fwd[0], v, fwd[1], s0_f)
    o_b, s_b = gla_chunk_scan(flip(q), flip(bwd[0]), flip(v), flip(bwd[1]), s0_b)
    return o_f + flip(o_b), s_f, s_b


def hgrn2_readout(o, g, norm_g, dtype):
    o = o * lax.rsqrt(jnp.mean(o * o, axis=-1, keepdims=True) + EPS) * norm_g.astype(F32)
    B, H, L, DV = o.shape
    o = o.transpose(0, 2, 1, 3).reshape(B, L, H * DV)
    return (o * jax.nn.silu(g.astype(F32))).astype(dtype)


def short_conv3(u, w, b):
    up = jnp.pad(u, ((0, 0), (1, 1), (0, 0)))
    return up[:, :-2] * w[0] + up[:, 1:-1] * w[1] + up[:, 2:] * w[2] + b


def hyena_filters(L, w1, b1, fr1, w2, b2, fr2, w3):
    t = jnp.linspace(0.0, 1.0, L, dtype=F32)[:, None]
    bands = (HY_EMB - 1) // 2
    ang = (2.0 * math.pi * jnp.arange(L, dtype=F32) / L)[:, None] * \
        jnp.linspace(1e-4, bands - 1, bands, dtype=F32)[None, :]
    z = jnp.concatenate([t, jnp.cos(ang), -jnp.sin(ang)], axis=-1)
    h = jnp.sin(fr1.astype(F32) * (z @ w1.astype(F32) + b1.astype(F32)))
    h = jnp.sin(fr2.astype(F32) * (h @ w2.astype(F32) + b2.astype(F32)))
    h = h @ w3.astype(F32)
    deltas = jnp.abs(jnp.linspace(math.log(HY_DECAY_TARGET) / HY_SLOW_PCT,
                                  math.log(HY_DECAY_TARGET) / HY_FAST_PCT, HY_WIDTH, dtype=F32))
    h = h.reshape(L, 2, HY_WIDTH) * jnp.exp(-t * deltas)[:, None, :]
    return h[:, 0], h[:, 1]


def long_conv_bidir(u, h_fwd, h_bwd, d_skip):
    B, L, C = u.shape
    kern = jnp.concatenate([h_fwd, jnp.zeros((1, C), F32), jnp.flip(h_bwd[1:], axis=0)], axis=0)
    y = jnp.fft.irfft(jnp.fft.rfft(u, n=2 * L, axis=1) * jnp.fft.rfft(kern, axis=0)[None],
                      n=2 * L, axis=1)[:, :L]
    return y + u * d_skip


def hyena_branch(p_hy, conv_w, conv_b, filt, d_skip):
    L = p_hy.shape[1]
    u = short_conv3(p_hy, conv_w, conv_b).astype(F32)
    v, x1, x0 = jnp.split(u, 3, axis=-1)
    h_fwd, h_bwd = hyena_filters(L, *filt)
    y = x0 * long_conv_bidir(v * x1, h_fwd, h_bwd, d_skip.astype(F32))
    return y.astype(p_hy.dtype)


def merge_branches(y_a, y_b, g_logits, w_pa, w_pb, w_o):
    g_a, g_b = jnp.split(g_logits, 2, axis=-1)
    mixed = jax.nn.sigmoid(g_a) * (y_a @ w_pa) + jax.nn.sigmoid(g_b) * (y_b @ w_pb)
    return mixed @ w_o


def token_mixer(p, lb, s0_f, s0_b, hg_norm_g, conv_w, conv_b, filt, d_skip, w_pa, w_pb, w_o):
    q, z_f, z_b, vi, g, p_hy, g_logits = jnp.split(p, SPLIT_POINTS, axis=-1)
    fwd = hgrn2_keys(z_f, lb[0])
    bwd = hgrn2_keys(z_b, lb[1])
    o, s_f, s_b = hgrn2_bidir(heads(q.astype(F32), HG_DK), heads(vi.astype(F32), HG_DV),
                              fwd, bwd, s0_f, s0_b)
    y_a = hgrn2_readout(o, g, hg_norm_g, p.dtype)
    y_b = hyena_branch(p_hy, conv_w, conv_b, filt, d_skip)
    return merge_branches(y_a, y_b, g_logits, w_pa, w_pb, w_o), s_f, s_b


def context_states(p_c, lb):
    z_f, z_b, vi = jnp.split(p_c, (HG_KDIM, 2 * HG_KDIM), axis=-1)
    kf, lf = hgrn2_keys(z_f, lb[0])
    kb, lb_ = hgrn2_keys(z_b, lb[1])
    v = heads(vi.astype(F32), HG_DV)
    return gla_final_state(kf, v, lf), gla_final_state(flip(kb), flip(v), flip(lb_))


def hier_moe(h, wg, bg, we, be, w_gate, w_up, w_down):
    B, L, D = h.shape
    t = h.reshape(B * L, D)
    p_group = jax.nn.softmax((t @ wg + bg).astype(F32), axis=-1)
    p_top_g, g_idx = lax.top_k(p_group, 1)
    e_logits = (jnp.einsum('td,gde->tge', t, we) + be).astype(F32)
    e_logits = jnp.take_along_axis(e_logits, g_idx[:, :, None], axis=1)[:, 0]
    p_top_e, e_idx = lax.top_k(jax.nn.softmax(e_logits, axis=-1), TOP_K)
    w_sel = p_top_g * p_top_e / jnp.sum(p_top_e, axis=-1, keepdims=True)
    combine = (jnp.einsum('tk,tke->te', w_sel, jax.nn.one_hot(e_idx, EXPERTS_PER_GROUP, dtype=F32))[:, None, :]
               * jax.nn.one_hot(g_idx[:, 0], N_GROUPS, dtype=F32)[:, :, None])
    out = jnp.zeros((B * L, D), F32)
    for gi in range(N_GROUPS):
        hid = (jax.nn.silu(jnp.einsum('td,edf->tef', t, w_gate[gi]))
               * jnp.einsum('td,edf->tef', t, w_up[gi])
               * combine[:, gi, :, None].astype(t.dtype))
        out = out + jnp.einsum('tef,efd->td', hid, w_down[gi])
    return out.reshape(B, L, D).astype(h.dtype)


def setup_inputs(seed: int = 0) -> dict:
    key = jax.random.key(seed)
    ks = jax.random.split(key, 32)

    def nrm(k, shape, scale):
        return scale * jax.random.normal(k, shape, F32)

    G, E, F = N_GROUPS, EXPERTS_PER_GROUP, D_EXPERT
    return {
        "x": nrm(ks[0], (BATCH, SEQ, D_MODEL), 1.0),
        "c": nrm(ks[1], (BATCH, D_MODEL), 1.0),
        "ctx": nrm(ks[2], (BATCH, CTX_LEN, D_MODEL), 1.0),
        "c_ctx": nrm(ks[3], (D_MODEL,), 1.0),
        "ada_w": nrm(ks[4], (DEPTH, D_MODEL, 6 * D_MODEL), 0.5 * D_MODEL ** -0.5),
        "ada_b": nrm(ks[5], (DEPTH, 6 * D_MODEL), 0.02),
        "norm1_g": 1.0 + nrm(ks[6], (DEPTH, D_MODEL), 0.05),
        "norm2_g": 1.0 + nrm(ks[7], (DEPTH, D_MODEL), 0.05),
        "w_in": nrm(ks[8], (DEPTH, D_MODEL, IN_WIDTH), D_MODEL ** -0.5),
        "hgrn_lb": nrm(ks[9], (DEPTH + 1, 2, HG_KDIM), 0.1),
        "hgrn_norm_g": 1.0 + nrm(ks[10], (DEPTH, HG_DV), 0.05),
        "hy_conv_w": nrm(ks[11], (DEPTH, HY_SHORT, 3 * HY_WIDTH), HY_SHORT ** -0.5),
        "hy_conv_b": nrm(ks[12], (DEPTH, 3 * HY_WIDTH), 0.02),
        "hy_filt_w1": nrm(ks[13], (DEPTH, HY_EMB, HY_FILTER_HIDDEN), HY_EMB ** -0.5),
        "hy_filt_b1": nrm(ks[14], (DEPTH, HY_FILTER_HIDDEN), 0.1),
        "hy_filt_freq1": 1.0 + nrm(ks[15], (DEPTH, HY_FILTER_HIDDEN), 0.1),
        "hy_filt_w2": nrm(ks[16], (DEPTH, HY_FILTER_HIDDEN, HY_FILTER_HIDDEN), HY_FILTER_HIDDEN ** -0.5),
        "hy_filt_b2": nrm(ks[17], (DEPTH, HY_FILTER_HIDDEN), 0.1),
        "hy_filt_freq2": 1.0 + nrm(ks[18], (DEPTH, HY_FILTER_HIDDEN), 0.1),
        "hy_filt_w3": nrm(ks[19], (DEPTH, HY_FILTER_HIDDEN, 2 * HY_WIDTH), HY_FILTER_SCALE * HY_FILTER_HIDDEN ** -0.5),
        "hy_d": nrm(ks[20], (DEPTH, HY_WIDTH), 0.1),
        "w_proj_a": nrm(ks[21], (DEPTH, HG_WIDTH, D_MODEL), HG_WIDTH ** -0.5),
        "w_proj_b": nrm(ks[22], (DEPTH, HY_WIDTH, D_MODEL), HY_WIDTH ** -0.5),
        "w_out": nrm(ks[23], (DEPTH, D_MODEL, D_MODEL), D_MODEL ** -0.5),
        "moe_router_g_w": nrm(ks[24], (DEPTH, D_MODEL, G), D_MODEL ** -0.5),
        "moe_router_g_b": nrm(ks[25], (DEPTH, G), 0.01),
        "moe_router_e_w": nrm(ks[26], (DEPTH, G, D_MODEL, E), D_MODEL ** -0.5),
        "moe_router_e_b": nrm(ks[27], (DEPTH, G, E), 0.01),
        "moe_w_gate": nrm(ks[28], (DEPTH, G, E, D_MODEL, F), D_MODEL ** -0.5),
        "moe_w_up": nrm(ks[29], (DEPTH, G, E, D_MODEL, F), D_MODEL ** -0.5),
        "moe_w_down": nrm(ks[30], (DEPTH, G, E, F, D_MODEL), F ** -0.5),
        "final_norm_g": 1.0 + nrm(ks[31], (D_MODEL,), 0.05),
    }


def reference(x, c, ctx, c_ctx, ada_w, ada_b, norm1_g, norm2_g, w_in, hgrn_lb, hgrn_norm_g,
              hy_conv_w, hy_conv_b, hy_filt_w1, hy_filt_b1, hy_filt_freq1, hy_filt_w2, hy_filt_b2,
              hy_filt_freq2, hy_filt_w3, hy_d, w_proj_a, w_proj_b, w_out, moe_router_g_w,
              moe_router_g_b, moe_router_e_w, moe_router_e_b, moe_w_gate, moe_w_up, moe_w_down,
              final_norm_g):
    B = x.shape[0]
    x = x + sincos_2d(x.shape[1]).astype(x.dtype)
    lbs = lower_bounds(hgrn_lb)
    zero_state = jnp.zeros((B, HG_HEADS, HG_DK, HG_DV), F32)
    for l in range(DEPTH):
        last = l == DEPTH - 1
        sh1, sc1, gt1, sh2, sc2, gt2 = adaln(c, ada_w[l], ada_b[l])
        csh1, csc1, cgt1, csh2, csc2, cgt2 = adaln(c_ctx, ada_w[l], ada_b[l])
        filt = (hy_filt_w1[l], hy_filt_b1[l], hy_filt_freq1[l], hy_filt_w2[l], hy_filt_b2[l],
                hy_filt_freq2[l], hy_filt_w3[l])
        mix_args = (hgrn_norm_g[l], hy_conv_w[l], hy_conv_b[l], filt, hy_d[l],
                    w_proj_a[l], w_proj_b[l], w_out[l])
        hc = modulate(rms_norm(ctx, norm1_g[l]), csh1, csc1)
        if last:
            s_f, s_b = context_states(hc @ w_in[l][:, HG_KDIM:3 * HG_KDIM + HG_WIDTH], lbs[l])
        else:
            ctx_mix, s_f, s_b = token_mixer(hc @ w_in[l], lbs[l], zero_state, zero_state, *mix_args)
        hx = modulate(rms_norm(x, norm1_g[l]), sh1, sc1)
        x_mix, _, _ = token_mixer(hx @ w_in[l], lbs[l], s_f, s_b, *mix_args)
        x = x + gt1 * x_mix
        moe_args = (moe_router_g_w[l], moe_router_g_b[l], moe_router_e_w[l], moe_router_e_b[l],
                    moe_w_gate[l], moe_w_up[l], moe_w_down[l])
        x = x + gt2 * hier_moe(modulate(rms_norm(x, norm2_g[l]), sh2, sc2), *moe_args)
        if not last:
            ctx = ctx + cgt1 * ctx_mix
            ctx = ctx + cgt2 * hier_moe(modulate(rms_norm(ctx, norm2_g[l]), csh2, csc2), *moe_args)
    return rms_norm(x, final_norm_g)
```

```python
import contextlib
import math
import numpy as np
import ml_dtypes
import concourse.bass as bass
import concourse.mybir as mybir
from concourse.bass_utils import run_bass_kernel_spmd

ACT = mybir.ActivationFunctionType
ALU = mybir.AluOpType
F32 = mybir.dt.float32
BF16 = mybir.dt.bfloat16
AX = mybir.AxisListType

NCORES = 8
D = 1024
L = 8192
NB = 2
EPS = 1e-6
NTILE = L // 128
TOK2 = 2048
MAGIC = 12582912.0
TWO_PI = 2.0 * math.pi


class Buf:
    __slots__ = ("name", "w", "r", "excl")

    def __init__(self, name="", excl=False):
        self.name = name
        self.w = None
        self.r = {}
        self.excl = excl


class TB:
    __slots__ = ("t", "b")

    def __init__(self, t, name=""):
        self.t = t
        self.b = Buf(name)


class Ctx:
    EPOCH = 30000
    NDMA = 32

    def __init__(self, nc, stack):
        self.nc = nc
        self.stack = stack
        self.eng = {"pe": nc.tensor, "act": nc.scalar, "dve": nc.vector,
                    "pool": nc.gpsimd, "sp": nc.sync}
        self.sems = {}
        self.cnt = {e: 0 for e in self.eng}
        self.seen = {e: {} for e in self.eng}
        self.dma_sems = [stack.enter_context(nc.semaphore(f"dq{i}")) for i in range(self.NDMA)]
        self.dma_cnt = [0] * self.NDMA
        self.dma_rr = 0
        self.n_wait = 0
        self.n_ins = 0
        self.extra_sems = []

    def _sem(self, key):
        s = self.sems.get(key)
        if s is None:
            s = self.stack.enter_context(self.nc.semaphore(f"s_{key[0]}_{key[1]}"))
            self.sems[key] = s
        return s

    def _wait(self, e, ev):
        if ev is None:
            return
        key, val = ev
        if self.seen[e].get(key, 0) >= val:
            return
        self.seen[e][key] = val
        sem = key if not isinstance(key, tuple) else self._sem(key)
        self.eng[e].wait_ge(sem, val)
        self.n_wait += 1

    def _deps(self, e, reads, writes):
        pe = (e == "pe")

        def w(ev):
            if ev is None:
                return
            if pe and isinstance(ev[0], tuple) and ev[0][0] == "pe":
                return
            self._wait(e, ev)
        for b in reads:
            w(b.w)
        for b in writes:
            w(b.w)
            for ev in list(b.r.items()):
                w(ev)

    def _commit(self, ev, reads, writes):
        for b in reads:
            if b.r.get(ev[0], 0) < ev[1]:
                b.r[ev[0]] = ev[1]
        for b in writes:
            b.w = ev
            b.r = {}

    def op(self, e, fn, reads=(), writes=()):
        writes = [b for b in writes if b is not None] + [b for b in reads if b is not None and b.excl]
        reads = [b for b in reads if b is not None and not b.excl]
        self._deps(e, reads, writes)
        self.cnt[e] += 1
        if self.cnt[e] % self.EPOCH == 0:
            self.cnt[e] += 1
        n = self.cnt[e]
        key = (e, n // self.EPOCH)
        val = n % self.EPOCH
        ins = fn(self.eng[e])
        ins.then_inc(self._sem(key), 1)
        ev = (key, val)
        self._commit(ev, reads, writes)
        self.n_ins += 1
        return ev

    def dma(self, out, in_, reads=(), writes=(), q="sp", **kw):
        reads = [b for b in reads if b is not None]
        writes = [b for b in writes if b is not None]
        self._deps(q, reads, writes)
        i = self.dma_rr
        self.dma_rr = (i + 1) % self.NDMA
        sem = self.dma_sems[i]
        if self.dma_cnt[i] > 0:
            self._wait(q, (sem, self.dma_cnt[i]))
        self.dma_cnt[i] += 16
        self.eng[q].dma_start(out=out, in_=in_, **kw).then_inc(sem, 16)
        ev = (sem, self.dma_cnt[i])
        self._commit(ev, reads, writes)
        self.n_ins += 1
        return ev

    def custom(self, e, emit, reads=(), writes=()):
        self._deps(e, list(reads), list(writes))
        sem = self.stack.enter_context(self.nc.semaphore(f"cs{len(self.extra_sems)}"))
        self.extra_sems.append(sem)
        emit(self.eng[e], sem)
        ev = (sem, 1)
        self._commit(ev, list(reads), list(writes))
        return ev

    def all_events(self):
        evs = []
        for e in self.eng:
            n = self.cnt[e]
            if n:
                evs.append(((e, n // self.EPOCH), n % self.EPOCH))
        for i in range(self.NDMA):
            if self.dma_cnt[i]:
                evs.append((self.dma_sems[i], self.dma_cnt[i]))
        for s in self.extra_sems:
            evs.append((s, 1))
        return evs

    def barrier(self, engines=("pe", "act", "dve", "pool", "sp")):
        evs = self.all_events()
        for e in engines:
            for ev in evs:
                if e == "pe" and isinstance(ev[0], tuple) and ev[0][0] == "pe":
                    continue
                self._wait(e, ev)


def _host_consts():
    i = np.arange(128)
    s = i[:, None]
    t = i[None, :]
    cf = np.zeros((128, 640), np.float32)
    cf[:, 0:128] = np.eye(128)
    cf[:, 128:256] = (s <= t)
    cf[:, 256:384] = (s >= t)
    cf[:, 384:512] = (s > t)
    cf[:, 512:640] = (s < t)
    cb = np.zeros((128, 11 * 128), np.float32)
    cb[:, 0:128] = np.eye(128)
    for k in range(3):
        cb[:, 128 * (1 + k):128 * (2 + k)] = (s == 127 - t + (k - 1))
        cb[:, 128 * (4 + k):128 * (5 + k)] = (s == t + (k - 1))
    e = np.zeros((128, 128), np.float32); e[127, 127] = 1; cb[:, 128 * 7:128 * 8] = e
    e = np.zeros((128, 128), np.float32); e[0, 0] = 1; cb[:, 128 * 8:128 * 9] = e
    e = np.zeros((128, 128), np.float32); e[127, 0] = 1; cb[:, 128 * 9:128 * 10] = e
    e = np.zeros((128, 128), np.float32); e[0, 127] = 1; cb[:, 128 * 10:128 * 11] = e
    quarter = D // 4
    omega = (1.0 / (np.float32(10000.0) ** (np.arange(quarter, dtype=np.float32) / np.float32(quarter)))).astype(np.float32)

    def axis_emb(pos):
        a = pos[:, None].astype(np.float32) * omega[None, :]
        return np.concatenate([np.sin(a), np.cos(a)], axis=-1).astype(np.float32)
    posr = axis_emb(np.arange(128))
    posc = axis_emb(np.arange(128) % 64)
    tl = np.linspace(0.0, 1.0, L, dtype=np.float32)[:, None]
    bands = 16
    ang = (np.float32(2.0 * math.pi) * np.arange(L, dtype=np.float32) / np.float32(L))[:, None] * \
        np.linspace(1e-4, bands - 1, bands, dtype=np.float32)[None, :]
    z = np.concatenate([tl, np.cos(ang), -np.sin(ang)], axis=-1).astype(np.float32)
    zT = np.ascontiguousarray(z.T)
    zTr = np.ascontiguousarray(z[::-1].T)
    deltas = np.abs(np.linspace(math.log(1e-2) / 1.5, math.log(1e-2) / 0.3, 512, dtype=np.float32)).astype(np.float32)
    cmask = np.concatenate([(s <= t), (s >= t)], axis=1).astype(np.int32)
    return dict(cf=cf, cb=cb.astype(ml_dtypes.bfloat16), posr=posr, posc=posc, zT=zT, zTr=zTr, deltas=deltas, cmask=cmask)


def _col_form(v, nch):
    return np.ascontiguousarray(np.asarray(v, np.float32).reshape(nch, 128).T)


def prep_inputs(inp):
    hc = _host_consts()
    f = lambda a: np.ascontiguousarray(np.asarray(a, dtype=np.float32))
    x = f(inp["x"]); c = f(inp["c"]); ctx = f(inp["ctx"]); c_ctx = f(inp["c_ctx"])
    w_in = f(inp["w_in"][0])
    xa = x.reshape(NB * L, D)
    ada_b = f(inp["ada_b"][0])
    common = dict(
        x_all=xa, posr=hc["posr"], posc=hc["posc"], ada_w=f(inp["ada_w"][0]),
        ada_bT=_col_form(ada_b, 48),
        ada_b_gt=np.ascontiguousarray(np.concatenate([ada_b[2048:3072], ada_b[5120:6144]])[None, :]),
        n1gT=_col_form(inp["norm1_g"][0], 8), n2gT=_col_form(inp["norm2_g"][0], 8),
        fng=f(inp["final_norm_g"])[None, :],
        w_gate=np.ascontiguousarray(w_in[:, 4096:6144]),
        w_pa=f(inp["w_proj_a"][0]), w_pb=f(inp["w_proj_b"][0]), w_out=f(inp["w_out"][0]),
        wr=np.ascontiguousarray(np.concatenate(
            [f(inp["moe_router_g_w"][0]), f(inp["moe_router_e_w"][0]).transpose(1, 0, 2).reshape(D, 32)], axis=1)),
        br=np.ascontiguousarray(np.concatenate([f(inp["moe_router_g_b"][0]), f(inp["moe_router_e_b"][0]).reshape(32)])[None, :]),
        moe_wg=f(inp["moe_w_gate"][0]).reshape(32, D, 512), moe_wu=f(inp["moe_w_up"][0]).reshape(32, D, 512),
        moe_wd=f(inp["moe_w_down"][0]).reshape(32, 512, D),
        cf=hc["cf"], cb=hc["cb"], zT=hc["zT"], zTr=hc["zTr"], cmask=hc["cmask"],
        fw1=f(inp["hy_filt_w1"][0]), fb1c=f(inp["hy_filt_b1"][0])[:, None], ffr1c=f(inp["hy_filt_freq1"][0])[:, None],
        fw2=f(inp["hy_filt_w2"][0]), fb2c=f(inp["hy_filt_b2"][0])[:, None], ffr2c=f(inp["hy_filt_freq2"][0])[:, None],
    )
    lb = f(inp["hgrn_lb"])
    cw = f(inp["hy_conv_w"][0])
    cbias = f(inp["hy_conv_b"][0])
    w3 = f(inp["hy_filt_w3"][0])
    hyd = f(inp["hy_d"][0])
    per = []
    for cid in range(NCORES):
        b, h = cid // 4, cid % 4
        hs = slice(h * 128, (h + 1) * 128)
        ch = np.arange(64 * cid, 64 * cid + 64)
        hycols = np.concatenate([2560 + ch, 2560 + 512 + ch, 2560 + 1024 + ch])
        hyc = np.concatenate([ch, 512 + ch, 1024 + ch])
        cv = np.stack([c[0], c[1], c_ctx, c[b]], 0)
        cvT = np.ascontiguousarray(cv.reshape(4, 8, 128).transpose(2, 1, 0).reshape(128, 32))
        d = dict(common)
        d.update(
            cvT=cvT, x_own=np.ascontiguousarray(x[b]), ctx_own=np.ascontiguousarray(ctx[b]),
            x_mine=np.ascontiguousarray(xa[TOK2 * cid:TOK2 * (cid + 1)]),
            posr_mine=np.ascontiguousarray(hc["posr"][32 * h:32 * h + 32]),
            w_hg=np.ascontiguousarray(np.concatenate(
                [w_in[:, 0 + h * 128:(h + 1) * 128], w_in[:, 512 + h * 128:512 + (h + 1) * 128],
                 w_in[:, 1024 + h * 128:1024 + (h + 1) * 128], w_in[:, 1536 + h * 128:1536 + (h + 1) * 128],
                 w_in[:, 2048 + h * 128:2048 + (h + 1) * 128]], axis=1)),
            w_hy=np.ascontiguousarray(w_in[:, hycols]),
            cw=np.ascontiguousarray(cw[:, hyc].reshape(1, 576)), cbias=np.ascontiguousarray(cbias[hyc][None, :]),
            fw3f=np.ascontiguousarray(w3[:, ch]), fw3b=np.ascontiguousarray(w3[:, 512 + ch]),
            hyd=np.ascontiguousarray(hyd[ch][:, None]), negdelta=np.ascontiguousarray(-hc["deltas"][ch][:, None]),
            lba=np.ascontiguousarray(np.stack([lb[0, 0, hs], lb[1, 0, hs], lb[0, 1, hs], lb[1, 1, hs]], 0).reshape(1, 512)),
            hgn=f(inp["hgrn_norm_g"][0])[None, :],
        )
        per.append(d)
    return per


class Prog:
    def __init__(self, debug=(), phases=("filter", "1a", "1b", "1c", "xchg", "2")):
        self.debug = set(debug)
        self.phases = set(phases)
        self.nc = bass.Bass("TRN2", target_bir_lowering=False)
        self.dbg_out = {}

    def din(self, name, shape, dtype=F32):
        return self.nc.dram_tensor(name, list(shape), dtype, kind="ExternalInput").ap()

    def sb(self, st, name, shape, dtype=F32):
        self._uid = getattr(self, "_uid", 0) + 1
        return TB(st.enter_context(self.nc.sbuf_tensor(f"s{self._uid}_{name}", list(shape), dtype)), name)

    def sbn(self, st, name, shape, dtype, n):
        return [self.sb(st, f"{name}{i}", shape, dtype) for i in range(n)]

    def dbg(self, name, shape, dtype=F32):
        t = self.nc.dram_tensor("dbg_" + name, list(shape), dtype, kind="ExternalOutput")
        self.dbg_out[name] = t
        return t.ap()

    def build(self):
        nc = self.nc
        with contextlib.ExitStack() as st:
            self.st = st
            cx = self.cx = Ctx(nc, st)
            self.declare_io()
            self.alloc_psum()
            ph = self.phases
            self.phase_consts(st)
            self.phase_adaln(st)
            cx.barrier()
            with contextlib.ExitStack() as hy_st:
                self.alloc_hyena_persist(hy_st)
                with contextlib.ExitStack() as fst:
                    fblocks = self.phase_filter(fst) if "filter" in ph else []
                    if "1a" in ph:
                        with contextlib.ExitStack() as pst:
                            self.phase_1a(pst, fblocks)
                            cx.barrier()
                    else:
                        for fb in fblocks:
                            fb()
                        if "gext" in self.debug:
                            cx.dma(self.dbg("gext", [64, 2 * L], BF16), self.gext.ap(), reads=[self.B_gext])
                    cx.barrier()
                for name, fn in (("1b", self.phase_1b), ("1c", self.phase_1c)):
                    if name in ph:
                        with contextlib.ExitStack() as pst:
                            fn(pst)
                            cx.barrier()
                        cx.barrier()
            if "xchg" in ph:
                self.phase_exchange()
            if "2" in ph:
                with contextlib.ExitStack() as pst:
                    self.phase_2(pst)
            for ev in cx.all_events():
                cx._wait("sp", ev)
        return nc

    IO = dict(
        x_all=([NB * L, D], F32), posr=([128, 512], F32), posc_d=([128, 512], F32, "posc"),
        ada_w=([D, 6 * D], F32), ada_bT_d=([128, 48], F32, "ada_bT"), ada_b_gt=([1, 2048], F32),
        n1gT_d=([128, 8], F32, "n1gT"), n2gT_d=([128, 8], F32, "n2gT"), fng=([1, D], F32),
        w_gate=([D, 2048], F32), w_pa=([512, D], F32), w_pb=([512, D], F32), w_out=([D, D], F32),
        wr=([D, 36], F32), br=([1, 36], F32),
        moe_wg=([32, D, 512], F32), moe_wu=([32, D, 512], F32), moe_wd=([32, 512, D], F32),
        cf_d=([128, 640], F32, "cf"), cb_d=([128, 11 * 128], BF16, "cb"), cmask_d=([128, 256], mybir.dt.int32, "cmask"),
        zT=([33, L], F32), zTr=([33, L], F32), fw1=([33, 64], F32), fb1c=([64, 1], F32), ffr1c=([64, 1], F32),
        fw2=([64, 64], F32), fb2c=([64, 1], F32), ffr2c=([64, 1], F32),
        cvT_d=([128, 32], F32, "cvT"), x_own=([L, D], F32), ctx_own=([256, D], F32),
        x_mine=([TOK2, D], F32), posr_mine=([32, 512], F32), w_hg=([D, 640], F32), w_hy=([D, 192], F32),
        cw_d=([1, 576], F32, "cw"), cbias_d=([1, 192], F32, "cbias"),
        fw3f=([64, 64], F32), fw3b=([64, 64], F32), hyd_d=([64, 1], F32, "hyd"),
        negdelta_d=([64, 1], F32, "negdelta"), lba=([1, 512], F32), hgn=([1, 128], F32),
    )

    def __getattr__(self, name):
        io = Prog.IO
        if name in io:
            spec = io[name]
            ap = self.din(spec[2] if len(spec) > 2 else name, spec[0], spec[1])
            self.used_inputs.append(spec[2] if len(spec) > 2 else name)
            setattr(self, name, ap)
            return ap
        raise AttributeError(name)

    def declare_io(self):
        self.used_inputs = []
        nc = self.nc
        self.out = nc.dram_tensor("out", [TOK2, D], F32, kind="ExternalOutput").ap()
        self.gt_scr = nc.dram_tensor("gt_scr", [1, 2048], F32)
        self.gext = nc.dram_tensor("gext", [64, 2 * L], BF16)
        self.ag_in = nc.dram_tensor("ag_in", [256, L], BF16)
        self.ag_out = nc.dram_tensor("ag_out", [NCORES * 256, L], BF16)
        self.x1_scr = nc.dram_tensor("x1_scr", [TOK2, D], F32)
        self.B_gt_scr = Buf("gt_scr"); self.B_gext = Buf("gext"); self.B_ag_in = Buf("ag_in"); self.B_ag_out = Buf("ag_out")
        self.B_x1_scr = [Buf(f"x1s{i}") for i in range(16)]

    def alloc_psum(self):
        self.PS = [TB(self.nc.alloc_psum_tensor(f"ps{i}", [128, 512], F32), f"ps{i}") for i in range(8)]
        for p in self.PS:
            p.b.excl = True

    def psb(self, i):
        return self.PS[i].t[:].bitcast(BF16)

    def phase_consts(self, st):
        cx = self.cx
        self.cf = self.sb(st, "cf", [128, 640]); self.cbf = self.sb(st, "cbf", [128, 11 * 128], BF16)
        self.posc = self.sb(st, "posc", [128, 512])
        cx.dma(self.cf.t[:], self.cf_d, writes=[self.cf.b])
        cx.dma(self.cbf.t[:], self.cb_d, writes=[self.cbf.b])
        cx.dma(self.posc.t[:], self.posc_d, writes=[self.posc.b])
        c = self.cf.t
        self.identF = c[:, 0:128]; self.tri = [c[:, 128:256], c[:, 256:384]]; self.triS = [c[:, 384:512], c[:, 512:640]]
        cb = self.cbf.t
        self.identB = cb[:, 0:128]
        self.Mrev = [cb[:, 128 * (1 + k):128 * (2 + k)] for k in range(3)]
        self.Mnat = [cb[:, 128 * (4 + k):128 * (5 + k)] for k in range(3)]
        self.Erev_prev = cb[:, 128 * 7:128 * 8]; self.Erev_next = cb[:, 128 * 8:128 * 9]
        self.Enat_prev = cb[:, 128 * 9:128 * 10]; self.Enat_next = cb[:, 128 * 10:128 * 11]

    def phase_adaln(self, st):
        cx, nc = self.cx, self.nc
        self.adaT = self.sb(st, "adaT", [128, 48, 4]); self.gsc1 = self.sb(st, "gsc1", [128, 8, 4])
        self.gsc2 = self.sb(st, "gsc2", [128, 8]); self.gtb = self.sb(st, "gtb", [128, 2048])
        with contextlib.ExitStack() as tst:
            cvT = self.sb(tst, "cvT", [128, 32]); scT = self.sb(tst, "scT", [128, 32])
            abT = self.sb(tst, "abT", [128, 48]); n1g = self.sb(tst, "n1g", [128, 8]); n2g = self.sb(tst, "n2g", [128, 8])
            abg = self.sb(tst, "abg", [4, 2048]); gtrow = self.sb(tst, "gtrow", [4, 2048])
            awb = self.sbn(tst, "awb", [128, 8, 512], F32, 3)
            cx.dma(cvT.t[:], self.cvT_d, writes=[cvT.b])
            cx.dma(abT.t[:], self.ada_bT_d, writes=[abT.b])
            cx.dma(n1g.t[:], self.n1gT_d, writes=[n1g.b])
            cx.dma(n2g.t[:], self.n2gT_d, writes=[n2g.b])
            cx.dma(abg.t[:], self.ada_b_gt.partition_broadcast(4), writes=[abg.b])
            cx.op("act", lambda e: e.activation(out=scT.t[:], in_=cvT.t[:], func=ACT.Silu), reads=[cvT.b], writes=[scT.b])
            colps = self.PS[0]; rowps = [self.PS[1], self.PS[2], self.PS[3], self.PS[4]]
            gtblk = {4: 0, 5: 1, 10: 2, 11: 3}
            aw_v = self.ada_w.rearrange("(k p) c -> p k c", p=128)
            for cbk in range(12):
                w = awb[cbk % 3]
                cx.dma(w.t[:], aw_v[:, :, cbk * 512:(cbk + 1) * 512], writes=[w.b], q=("sp" if cbk % 2 == 0 else "pool"))

                def mm(e, cbk=cbk, w=w):
                    last = None
                    for c4 in range(4):
                        ch = 4 * cbk + c4
                        for k in range(8):
                            last = e.matmul(colps.t[:, ch * 4:(ch + 1) * 4], lhsT=w.t[:, k, c4 * 128:(c4 + 1) * 128],
                                            rhs=scT.t[:, k * 4:(k + 1) * 4], start=(k == 0), stop=(k == 7))
                    if cbk in gtblk:
                        rp = rowps[gtblk[cbk]]
                        for k in range(8):
                            last = e.matmul(rp.t[0:4, :], lhsT=scT.t[:, k * 4:(k + 1) * 4], rhs=w.t[:, k, :],
                                            start=(k == 0), stop=(k == 7))
                    return last
                wr = [colps.b] + ([rowps[gtblk[cbk]].b] if cbk in gtblk else [])
                cx.op("pe", mm, reads=[w.b, scT.b], writes=wr)
            adaT = self.adaT
            cx.op("dve", lambda e: e.tensor_tensor(out=adaT.t[:], in0=colps.t[:, 0:192].rearrange("p (c j) -> p c j", j=4),
                                                   in1=abT.t[:].unsqueeze(2).to_broadcast([128, 48, 4]), op=ALU.add),
                  reads=[colps.b, abT.b], writes=[adaT.b])
            cx.op("dve", lambda e: e.scalar_tensor_tensor(out=self.gsc1.t[:], in0=adaT.t[:, 8:16, :], scalar=1.0,
                                                          in1=n1g.t[:].unsqueeze(2).to_broadcast([128, 8, 4]),
                                                          op0=ALU.add, op1=ALU.mult),
                  reads=[adaT.b, n1g.b], writes=[self.gsc1.b])
            cx.op("dve", lambda e: e.scalar_tensor_tensor(out=self.gsc2.t[:], in0=adaT.t[:, 32:40, 3], scalar=1.0,
                                                          in1=n2g.t[:], op0=ALU.add, op1=ALU.mult),
                  reads=[adaT.b, n2g.b], writes=[self.gsc2.b])
            for i in range(4):
                cx.op("dve", lambda e, i=i: e.tensor_tensor(out=gtrow.t[:, i * 512:(i + 1) * 512], in0=rowps[i].t[0:4, :],
                                                           in1=abg.t[:, i * 512:(i + 1) * 512], op=ALU.add),
                      reads=[rowps[i].b, abg.b], writes=[gtrow.b])
            cx.dma(self.gt_scr.ap(), gtrow.t[3:4, :], reads=[gtrow.b], writes=[self.B_gt_scr])
            cx.dma(self.gtb.t[:], self.gt_scr.ap().partition_broadcast(128), reads=[self.B_gt_scr], writes=[self.gtb.b])
            cx.barrier()

    def sh1(self, k, j):
        return self.adaT.t[:, k, j:j + 1]

    def sh2(self, k):
        return self.adaT.t[:, 24 + k, 3:4]

    def alloc_norm(self, st, nslot=3, nxn=2):
        self.XT = self.sbn(st, "xt", [128, D], F32, nslot)
        self.PT = self.sbn(st, "pt", [128, 512], F32, nslot)
        self.XN = self.sbn(st, "xn", [128, D], BF16, nxn)
        self.JK = self.sb(st, "junk", [128, D], BF16)
        self.SS = self.sbn(st, "ss", [128, 4], F32, 6)
        self.norm_i = 0

    def norm_load(self, src, pos):
        cx = self.cx
        i = self.norm_i
        self.norm_i += 1
        xt = self.XT[i % len(self.XT)]; pt = self.PT[i % len(self.PT)]
        cx.dma(xt.t[:], src, writes=[xt.b])
        if pos is not None:
            cx.dma(pt.t[0:64, :], pos[0].partition_broadcast(64), writes=[pt.b])
            cx.dma(pt.t[64:128, :], pos[1].partition_broadcast(64), writes=[pt.b])
        return dict(i=i, xt=xt, pt=pt, pos=pos is not None)

    def norm_stats(self, stt):
        cx = self.cx
        i, xt, pt = stt["i"], stt["xt"], stt["pt"]
        xn = self.XN[i % len(self.XN)]; ss = self.SS[i % len(self.SS)]
        stt["xn"] = xn
        if stt["pos"]:
            cx.op("pool", lambda e: e.tensor_tensor(out=xt.t[:, 0:512], in0=xt.t[:, 0:512], in1=pt.t[:], op=ALU.add),
                  reads=[xt.b, pt.b], writes=[xt.b])
            cx.op("pool", lambda e: e.tensor_tensor(out=xt.t[:, 512:1024], in0=xt.t[:, 512:1024], in1=self.posc.t[:], op=ALU.add),
                  reads=[xt.b], writes=[xt.b])
        cx.op("act", lambda e: e.activation(out=self.JK.t[:], in_=xt.t[:], func=ACT.Square, accum_out=ss.t[:, 0:1]),
              reads=[xt.b], writes=[ss.b, self.JK.b])
        cx.op("act", lambda e: e.activation(out=ss.t[:, 1:2], in_=ss.t[:, 0:1], func=ACT.Sqrt, scale=1.0 / D, bias=EPS),
              reads=[ss.b], writes=[ss.b])
        cx.op("dve", lambda e: e.reciprocal(out=ss.t[:, 2:3], in_=ss.t[:, 1:2]), reads=[ss.b], writes=[ss.b])
        cx.op("dve", lambda e: e.tensor_scalar(out=xn.t[:], in0=xt.t[:], scalar1=ss.t[:, 2:3], scalar2=None, op0=ALU.mult),
              reads=[xt.b, ss.b], writes=[xn.b])

    def norm_tr(self, stt, jmod, hxT, ps_bank):
        cx = self.cx
        xn = stt["xn"]
        pb = self.PS[ps_bank]
        pv = self.psb(ps_bank)

        def tr(e):
            last = None
            for k in range(8):
                last = e.transpose(pv[:, k * 128:(k + 1) * 128], xn.t[:, k * 128:(k + 1) * 128], self.identB)
            return last
        cx.op("pe", tr, reads=[xn.b], writes=[pb.b])
        for k in range(8):
            sc = self.gsc1.t[:, k, jmod:jmod + 1]
            bi = self.sh1(k, jmod)
            cx.op("act", lambda e, k=k, sc=sc, bi=bi: e.activation(out=hxT.t[:, k, :], in_=pv[:, k * 128:(k + 1) * 128],
                                                                  func=ACT.Identity, scale=sc, bias=bi),
                  reads=[pb.b], writes=[hxT.b])

    def norm_tile(self, src, pos, jmod, hxT, ps_bank):
        stt = self.norm_load(src, pos)
        self.norm_stats(stt)
        self.norm_tr(stt, jmod, hxT, ps_bank)

    def alloc_hyena_persist(self, st):
        self.Wp = self.sb(st, "Wp", [128, 64, 128], BF16)
        self.X0 = self.sb(st, "X0", [128, 64, 128], BF16)
        self.Wp_b = [Buf(f"wp{n}") for n in range(128)]
        self.X0_b = [Buf(f"x0{n}") for n in range(128)]

    def phase_filter(self, st):
        cx = self.cx
        w1 = self.sb(st, "fw1", [33, 64]); w2 = self.sb(st, "fw2", [64, 64])
        w3 = [self.sb(st, "fw3f", [64, 64]), self.sb(st, "fw3b", [64, 64])]
        prm = self.sb(st, "fprm", [64, 8])
        cx.dma(w1.t[:], self.fw1, writes=[w1.b]); cx.dma(w2.t[:], self.fw2, writes=[w2.b])
        cx.dma(w3[0].t[:], self.fw3f, writes=[w3[0].b]); cx.dma(w3[1].t[:], self.fw3b, writes=[w3[1].b])
        for i, src in enumerate([self.fb1c, self.ffr1c, self.fb2c, self.ffr2c, self.hyd_d, self.negdelta_d]):
            cx.dma(prm.t[:, i:i + 1], src, writes=[prm.b])
        cx.op("dve", lambda e: e.tensor_tensor(out=prm.t[:, 6:7], in0=prm.t[:, 0:1], in1=prm.t[:, 1:2], op=ALU.mult),
              reads=[prm.b], writes=[prm.b])
        cx.op("dve", lambda e: e.tensor_tensor(out=prm.t[:, 7:8], in0=prm.t[:, 2:3], in1=prm.t[:, 3:4], op=ALU.mult),
              reads=[prm.b], writes=[prm.b])
        W = 512
        BW = 2048
        z = self.sb(st, "fz", [33, BW]); tt = self.sb(st, "ft", [64, BW])
        a1 = self.sb(st, "fa", [64, BW]); r1 = self.sb(st, "fr", [64, BW])
        h1 = self.sb(st, "fh1", [64, BW]); h2 = self.sb(st, "fh2", [64, BW])
        dc = self.sb(st, "fdc", [64, BW]); gf = self.sb(st, "fgf", [64, W])
        go = self.sbn(st, "fgo", [64, BW], BF16, 2)
        pbk = [self.PS[5], self.PS[6], self.PS[7]]
        cnt = [0]

        def layer(wt, frc, fbc, src, hout):
            for sub in range(BW // W):
                ps = pbk[cnt[0] % 3]; cnt[0] += 1
                sl = slice(sub * W, (sub + 1) * W)
                cx.op("pe", lambda e, ps=ps, sl=sl: e.matmul(ps.t[0:64, :], lhsT=wt.t[:], rhs=src.t[:, sl], start=True, stop=True),
                      reads=[wt.b, src.b], writes=[ps.b])
                cx.op("dve", lambda e, ps=ps, sl=sl: e.tensor_scalar(out=a1.t[:, sl], in0=ps.t[0:64, :], scalar1=frc, scalar2=fbc,
                                                                 op0=ALU.mult, op1=ALU.add), reads=[ps.b, prm.b], writes=[a1.b])
            cx.op("dve", lambda e: e.tensor_scalar(out=r1.t[:], in0=a1.t[:], scalar1=1.0 / TWO_PI, scalar2=MAGIC,
                                                   op0=ALU.mult, op1=ALU.add), reads=[a1.b], writes=[r1.b])
            cx.op("dve", lambda e: e.tensor_scalar(out=r1.t[:], in0=r1.t[:], scalar1=-MAGIC, scalar2=None, op0=ALU.add),
                  reads=[r1.b], writes=[r1.b])
            cx.op("dve", lambda e: e.scalar_tensor_tensor(out=a1.t[:], in0=r1.t[:], scalar=-TWO_PI, in1=a1.t[:],
                                                          op0=ALU.mult, op1=ALU.add), reads=[r1.b, a1.b], writes=[a1.b])
            cx.op("dve", lambda e: e.tensor_scalar(out=a1.t[:], in0=a1.t[:], scalar1=math.pi, scalar2=-math.pi,
                                                   op0=ALU.min, op1=ALU.max), reads=[a1.b], writes=[a1.b])
            cx.op("act", lambda e: e.activation(out=hout.t[:], in_=a1.t[:], func=ACT.Sin), reads=[a1.b], writes=[hout.b])

        def blockfn(dr, blk):
            zsrc = self.zT if dr == 0 else self.zTr
            gob = go[(dr * 4 + blk) % 2]
            c0 = blk * BW
            cx.dma(z.t[:], zsrc[:, c0:c0 + BW], writes=[z.b])
            cx.dma(tt.t[:], zsrc[0:1, c0:c0 + BW].partition_broadcast(64), writes=[tt.b])
            cx.op("act", lambda e: e.activation(out=dc.t[:], in_=tt.t[:], func=ACT.Exp, scale=prm.t[:, 5:6]),
                  reads=[tt.b, prm.b], writes=[dc.b])
            layer(w1, prm.t[:, 1:2], prm.t[:, 6:7], z, h1)
            layer(w2, prm.t[:, 3:4], prm.t[:, 7:8], h1, h2)
            for sub in range(BW // W):
                ps = pbk[cnt[0] % 3]; cnt[0] += 1
                sl = slice(sub * W, (sub + 1) * W)
                cx.op("pe", lambda e, ps=ps, sl=sl: e.matmul(ps.t[0:64, :], lhsT=w3[dr].t[:], rhs=h2.t[:, sl], start=True, stop=True),
                      reads=[w3[dr].b, h2.b], writes=[ps.b])
                if dr == 0 and c0 == 0 and sub == 0:
                    cx.op("dve", lambda e, ps=ps, sl=sl: e.tensor_tensor(out=gf.t[:], in0=ps.t[0:64, :], in1=dc.t[:, sl], op=ALU.mult),
                          reads=[ps.b, dc.b], writes=[gf.b])
                    cx.op("dve", lambda e: e.tensor_tensor(out=gf.t[:, 0:1], in0=gf.t[:, 0:1], in1=prm.t[:, 4:5], op=ALU.add),
                          reads=[gf.b, prm.b], writes=[gf.b])
                    cx.op("dve", lambda e, sl=sl: e.tensor_copy(out=gob.t[:, sl], in_=gf.t[:]), reads=[gf.b], writes=[gob.b])
                else:
                    cx.op("dve", lambda e, ps=ps, sl=sl: e.tensor_tensor(out=gob.t[:, sl], in0=ps.t[0:64, :], in1=dc.t[:, sl], op=ALU.mult),
                          reads=[ps.b, dc.b], writes=[gob.b])
            ge = self.gext.ap()
            if dr == 0:
                cx.dma(ge[:, L + blk * BW:L + (blk + 1) * BW], gob.t[:], reads=[gob.b], writes=[self.B_gext])
            else:
                n = BW if blk < 3 else BW - 1
                cx.dma(ge[:, 1 + blk * BW:1 + blk * BW + n], gob.t[:, 0:n], reads=[gob.b], writes=[self.B_gext])

        blocks = []
        for dr in range(2):
            for blk in range(L // BW):
                blocks.append(lambda dr=dr, blk=blk: blockfn(dr, blk))
        return blocks

    def phase_1a(self, st, fblocks=()):
        cx = self.cx
        fblocks = list(fblocks)
        ones = self.sb(st, "ones1a", [1, 128], BF16)
        cx.op("pool", lambda e: e.memset(ones.t[:], 1.0), writes=[ones.b])
        whyb = [self.fold_weights(st, f"why{b}", self.w_hy, 192, b) for b in range(2)]
        self.alloc_norm(st, nslot=3, nxn=3)
        cwb = self.sb(st, "cwb", [128, 3, 192]); cbb = self.sb(st, "cbb", [128, 192])
        cx.dma(cwb.t[:].rearrange("p a c -> p (a c)"), self.cw_d.partition_broadcast(128), writes=[cwb.b])
        cx.dma(cbb.t[:], self.cbias_d.partition_broadcast(128), writes=[cbb.b])
        HX = self.sbn(st, "hxa", [128, 8, 128], BF16, 3)
        P = self.sbn(st, "pk", [128, 3, 192], BF16, 4)
        U = self.sbn(st, "u", [128, 192], F32, 2)
        NT = NB * NTILE

        def conv_tile(m):
            j = m % NTILE
            pc = self.PS[4]
            cur = P[m % 4]
            prv = P[(m - 1) % 4] if j > 0 else None
            nxt = P[(m + 1) % 4] if j < NTILE - 1 else None

            def mm(e):
                ops = [(self.Mrev[k], cur.t[:, k, 0:128]) for k in range(3)]
                if prv is not None:
                    ops.append((self.Erev_prev, prv.t[:, 0, 0:128]))
                if nxt is not None:
                    ops.append((self.Erev_next, nxt.t[:, 2, 0:128]))
                for i, (lh, rh) in enumerate(ops):
                    e.matmul(pc.t[:, 0:128], lhsT=lh, rhs=rh, start=(i == 0), stop=(i == len(ops) - 1))
                ops = [(self.Mnat[k], cur.t[:, k, 128:192]) for k in range(3)]
                if prv is not None:
                    ops.append((self.Enat_prev, prv.t[:, 0, 128:192]))
                if nxt is not None:
                    ops.append((self.Enat_next, nxt.t[:, 2, 128:192]))
                last = None
                for i, (lh, rh) in enumerate(ops):
                    last = e.matmul(pc.t[:, 128:192], lhsT=lh, rhs=rh, start=(i == 0), stop=(i == len(ops) - 1))
                return last
            rd = [cur.b] + ([prv.b] if prv is not None else []) + ([nxt.b] if nxt is not None else [])
            cx.op("pe", mm, reads=rd, writes=[pc.b])
            u = U[m % 2]
            cx.op("dve", lambda e: e.tensor_tensor(out=u.t[:], in0=pc.t[:, 0:192], in1=cbb.t[:], op=ALU.add),
                  reads=[pc.b, cbb.b], writes=[u.b])
            cx.op("pool", lambda e: e.tensor_tensor(out=self.Wp.t[:, :, m], in0=u.t[:, 0:64], in1=u.t[:, 64:128], op=ALU.mult),
                  reads=[u.b], writes=[self.Wp_b[m]])
            cx.op("act", lambda e: e.copy(out=self.X0.t[:, :, m], in_=u.t[:, 128:192]), reads=[u.b], writes=[self.X0_b[m]])

        states = {}

        def s0(n):
            j = n % NTILE
            states[n] = self.norm_load(self.x_all[n * 128:(n + 1) * 128, :],
                                       (self.posr[2 * j:2 * j + 1, :], self.posr[2 * j + 1:2 * j + 2, :]))

        def s1(n):
            self.norm_stats(states[n])

        def s2(n):
            self.norm_tr_plain(states[n], HX[n % 3], ps_bank=(n % 2), eng=("act" if n % 2 == 0 else "dve"))

        def s3(n):
            hx = HX[n % 3]
            pp = self.PS[2 + (n % 2)]
            why, shw = whyb[n // NTILE]

            def mm(e, hx=hx, pp=pp):
                for k in range(8):
                    e.matmul(pp.t[:, 0:192], lhsT=hx.t[:, k, :], rhs=why.t[:, k, :], start=(k == 0), stop=False)
                return e.matmul(pp.t[:, 0:192], lhsT=ones.t[0:1, :], rhs=shw.t[0:1, :], start=False, stop=True)
            cx.op("pe", mm, reads=[hx.b, why.b, shw.b, ones.b], writes=[pp.b])
            pk = P[n % 4]
            cx.op("dve", lambda e, pk=pk, pp=pp: e.tensor_tensor(out=pk.t[:], in0=pp.t[:, 0:192].unsqueeze(1).to_broadcast([128, 3, 192]),
                                                               in1=cwb.t[:], op=ALU.mult),
                  reads=[pp.b, cwb.b], writes=[pk.b])
            states.pop(n, None)

        stages = [(5, conv_tile), (3, s3), (2, s2), (1, s1), (0, s0)]
        for i in range(NT + 5):
            for off, fn in stages:
                n = i - off
                if 0 <= n < NT:
                    fn(n)
            if fblocks and i % 12 == 1:
                fblocks.pop(0)()
        while fblocks:
            fblocks.pop(0)()
        if "gext" in self.debug:
            cx.dma(self.dbg("gext", [64, 2 * L], BF16), self.gext.ap(), reads=[self.B_gext])
        if "wp" in self.debug:
            cx.dma(self.dbg("wp", [128, 64 * 128], BF16), self.Wp.t[:].rearrange("p c n -> p (c n)"), reads=self.Wp_b)
            cx.dma(self.dbg("x0", [128, 64 * 128], BF16), self.X0.t[:].rearrange("p c n -> p (c n)"), reads=self.X0_b)

    def fold_weights(self, st, name, wdram, C, jmod):
        cx = self.cx
        Wb = self.sb(st, name, [128, 8, C], BF16); shW = self.sb(st, name + "_sh", [1, C], BF16)
        with contextlib.ExitStack() as ts:
            Wf = self.sb(ts, name + "_f", [128, 8, C], F32)
            cx.dma(Wf.t[:], wdram.rearrange("(k p) c -> p k c", p=128), writes=[Wf.b])
            for k in range(8):
                sc = self.gsc1.t[:, k, jmod:jmod + 1]
                if k % 2 == 0:
                    cx.op("act", lambda e, k=k, sc=sc: e.activation(out=Wb.t[:, k, :], in_=Wf.t[:, k, :], func=ACT.Copy, scale=sc),
                          reads=[Wf.b, self.gsc1.b], writes=[Wb.b])
                else:
                    cx.op("dve", lambda e, k=k, sc=sc: e.tensor_scalar(out=Wb.t[:, k, :], in0=Wf.t[:, k, :], scalar1=sc, scalar2=None, op0=ALU.mult),
                          reads=[Wf.b, self.gsc1.b], writes=[Wb.b])
            ps = self.PS[7]
            for c0 in range(0, C, 512):
                cw = min(512, C - c0)

                def mm(e, c0=c0, cw=cw):
                    last = None
                    for k in range(8):
                        last = e.matmul(ps.t[0:1, 0:cw], lhsT=self.adaT.t[:, k, jmod:jmod + 1], rhs=Wf.t[:, k, c0:c0 + cw],
                                        start=(k == 0), stop=(k == 7))
                    return last
                cx.op("pe", mm, reads=[Wf.b, self.adaT.b], writes=[ps.b])
                cx.op("act", lambda e, c0=c0, cw=cw: e.copy(out=shW.t[0:1, c0:c0 + cw], in_=ps.t[0:1, 0:cw]), reads=[ps.b], writes=[shW.b])
            cx.barrier()
        return Wb, shW

    def norm_tr_plain(self, stt, hxT, ps_bank, eng):
        cx = self.cx
        xn = stt["xn"]
        pb = self.PS[ps_bank]
        pv = self.psb(ps_bank)

        def tr(e):
            last = None
            for k in range(8):
                last = e.transpose(pv[:, k * 128:(k + 1) * 128], xn.t[:, k * 128:(k + 1) * 128], self.identB)
            return last
        cx.op("pe", tr, reads=[xn.b], writes=[pb.b])
        dst = hxT.t[:].rearrange("p k t -> p (k t)")
        if eng == "act":
            cx.op("act", lambda e: e.copy(out=dst, in_=pv[:, 0:1024]), reads=[pb.b], writes=[hxT.b])
        else:
            cx.op("dve", lambda e: e.tensor_copy(out=dst, in_=pv[:, 0:1024]), reads=[pb.b], writes=[hxT.b])

    def phase_1b(self, st):
        cx = self.cx
        ones = self.sb(st, "ones1", [1, 128], BF16)
        cx.op("pool", lambda e: e.memset(ones.t[:], 1.0), writes=[ones.b])
        whg, shg = self.fold_weights(st, "whg", self.w_hg, 640, 3)
        whc, shc = self.fold_weights(st, "whc", self.w_hg[:, 128:512], 384, 2)
        self.alloc_norm(st, nslot=2, nxn=2)
        lbt = self.sb(st, "lbt", [128, 4, 128]); oml = self.sb(st, "oml", [128, 2, 128]); hgnb = self.sb(st, "hgnb", [128, 128])
        cx.dma(lbt.t[:].rearrange("p a c -> p (a c)"), self.lba.partition_broadcast(128), writes=[lbt.b])
        cx.dma(hgnb.t[:], self.hgn.partition_broadcast(128), writes=[hgnb.b])
        for dr in range(2):
            cx.op("dve", lambda e, dr=dr: e.tensor_tensor(out=oml.t[:, dr, :], in0=lbt.t[:, 2 * dr, :], in1=lbt.t[:, 2 * dr + 1, :],
                                                        op=ALU.subtract), reads=[lbt.b], writes=[oml.b])
        cx.op("act", lambda e: e.activation(out=oml.t[:], in_=oml.t[:], func=ACT.Exp), reads=[oml.b], writes=[oml.b])
        cx.op("dve", lambda e: e.tensor_scalar(out=oml.t[:], in0=oml.t[:], scalar1=1.0, scalar2=None, op0=ALU.add),
              reads=[oml.b], writes=[oml.b])
        cx.op("dve", lambda e: e.reciprocal(out=oml.t[:], in_=oml.t[:]), reads=[oml.b], writes=[oml.b])

        HX = self.sbn(st, "hxb", [128, 8, 128], BF16, 3)
        OACC = self.sb(st, "oacc", [128, 64, 128]); KVB = self.sb(st, "kvb", [128, 66, 128], BF16)
        QB = self.sb(st, "qb", [128, 64, 128], BF16); SG = self.sb(st, "sg", [128, 64, 128], BF16)
        ETB = self.sb(st, "etb", [128, 66]); YAT = self.sbn(st, "yat", [128, 1024], BF16, 2)
        OACC_b = [Buf() for _ in range(64)]; KVB_b = [Buf() for _ in range(66)]; QB_b = [Buf() for _ in range(64)]
        SG_b = [Buf() for _ in range(64)]; ETB_b = [Buf() for _ in range(66)]
        Sf = self.sb(st, "Sf", [128, 128]); Sfb = self.sb(st, "Sfb", [128, 128], BF16)
        Sb = self.sb(st, "Sb", [128, 128]); Sbb = self.sb(st, "Sbb", [128, 128], BF16)

        def mk(nm, dt=F32, w=128, ns=2):
            return [[self.sb(st, f"{nm}{d}{s}", [128, w], dt) for s in range(ns)] for d in range(2)]
        EZ = mk("ez"); KK = mk("kk"); LF = mk("lf"); NR = mk("nr", F32, 2); AA = mk("aa"); BI = mk("bi"); EB = mk("eb", ns=3); ER = mk("er")
        QPP = mk("qpp", BF16); KPP = mk("kpp", BF16); KP = mk("kp", BF16); STM = mk("stm", BF16)
        MSK = self.sb(st, "msk", [128, 256], mybir.dt.int32)
        for d_ in range(2):
            cx.op("dve", lambda e, d_=d_: e.tensor_copy(out=MSK.t[:, d_ * 128:(d_ + 1) * 128], in_=self.tri[d_]),
                  reads=[self.cf.b], writes=[MSK.b])
        for d_ in range(2):
            for s_ in range(2):
                cx.op("pool", lambda e, t_=STM[d_][s_]: e.memset(t_.t[:], 0.0), writes=[STM[d_][s_].b])
        QPF = self.sbn(st, "qpf", [128, 128], BF16, 3); VT = self.sbn(st, "vt", [128, 128], BF16, 5)
        SGR = self.sbn(st, "sgr", [128, 128], F32, 2); QT = self.sbn(st, "qts", [128, 128], F32, 3)
        R = lambda bank, i: (self.PS[bank].t[:, i * 128:(i + 1) * 128])
        pz = self.PS[1]; PZb = self.PS[1].b
        pq = R(2, 0); po2 = R(2, 1); Bq = self.PS[2].b; Bo2 = self.PS[2].b
        BD = [self.PS[3].b, self.PS[4].b]
        pbT = [R(3, 0), R(4, 0)]; prem = [R(3, 1), R(4, 1)]; pkT = [R(3, 2), R(4, 2)]
        psT = [R(5, 0), R(5, 1)]; BsT = self.PS[5].b
        pKV = [R(6, 0), R(6, 1)]; BKV = self.PS[6].b
        po = R(7, 0); Bo = self.PS[7].b
        cx.op("pool", lambda e: e.memset(Sf.t[:], 0.0), writes=[Sf.b])
        cx.op("pool", lambda e: e.memset(Sb.t[:], 0.0), writes=[Sb.b])
        cx.op("pool", lambda e: e.memset(Sfb.t[:], 0.0), writes=[Sfb.b])

        NTT = 66
        st8 = {}

        def is_ctx(t):
            return t < 2

        def s0(t):
            if is_ctx(t):
                st8[t] = self.norm_load(self.ctx_own[t * 128:(t + 1) * 128, :], None)
            else:
                j = t - 2
                st8[t] = self.norm_load(self.x_own[j * 128:(j + 1) * 128, :],
                                        (self.posr[2 * j:2 * j + 1, :], self.posr[2 * j + 1:2 * j + 2, :]))

        def s1(t):
            self.norm_stats(st8[t])

        def s2(t):
            self.norm_tr_plain(st8[t], HX[t % 3], ps_bank=0, eng=("act" if t % 2 == 0 else "dve"))

        def s3(t):
            hx = HX[t % 3]; vt = VT[t % 5]
            if is_ctx(t):
                def mmz(e):
                    for k in range(8):
                        e.matmul(pz.t[:, 0:384], lhsT=hx.t[:, k, :], rhs=whc.t[:, k, :], start=(k == 0), stop=False)
                    return e.matmul(pz.t[:, 0:384], lhsT=ones.t[0:1, :], rhs=shc.t[0:1, :], start=False, stop=True)
                cx.op("pe", mmz, reads=[hx.b, whc.b, shc.b, ones.b], writes=[PZb])
            else:
                def mmz(e):
                    for k in range(8):
                        e.matmul(pz.t[:, 0:512], lhsT=hx.t[:, k, :], rhs=whg.t[:, k, 128:640], start=(k == 0), stop=False)
                    e.matmul(pz.t[:, 0:512], lhsT=ones.t[0:1, :], rhs=shg.t[0:1, 128:640], start=False, stop=True)
                    for k in range(8):
                        e.matmul(pq, lhsT=whg.t[:, k, 0:128], rhs=hx.t[:, k, :], start=(k == 0), stop=False)
                    return e.matmul(pq, lhsT=shg.t[0:1, 0:128], rhs=ones.t[0:1, :], start=False, stop=True)
                cx.op("pe", mmz, reads=[hx.b, whg.b, shg.b, ones.b], writes=[PZb, Bq])
            for dr in range(2):
                ez = EZ[dr][t % 2]
                cx.op("act", lambda e, dr=dr, ez=ez: e.activation(out=ez.t[:], in_=pz.t[:, dr * 128:(dr + 1) * 128], func=ACT.Exp),
                      reads=[PZb], writes=[ez.b])
            cx.op("act", lambda e: e.copy(out=vt.t[:], in_=pz.t[:, 256:384]), reads=[PZb], writes=[vt.b])
            if not is_ctx(t):
                sgr = SGR[t % 2]; qt = QT[t % 3]
                cx.op("act", lambda e: e.activation(out=sgr.t[:], in_=pz.t[:, 384:512], func=ACT.Silu), reads=[PZb], writes=[sgr.b])
                cx.op("dve", lambda e: e.tensor_copy(out=qt.t[:], in_=pq), reads=[Bq], writes=[qt.b])

        def s4(t):
            for dr in range(2):
                ez, kk = EZ[dr][t % 2], KK[dr][t % 2]
                cx.op("dve", lambda e, ez=ez: e.tensor_scalar(out=ez.t[:], in0=ez.t[:], scalar1=1.0, scalar2=None, op0=ALU.add),
                      reads=[ez.b], writes=[ez.b])
                cx.op("dve", lambda e, ez=ez: e.reciprocal(out=ez.t[:], in_=ez.t[:]), reads=[ez.b], writes=[ez.b])
                cx.op("dve", lambda e, ez=ez, kk=kk, dr=dr: e.tensor_tensor(out=kk.t[:], in0=ez.t[:], in1=oml.t[:, dr, :], op=ALU.mult),
                      reads=[ez.b, oml.b], writes=[kk.b])
            for dr in range(2):
                kk, lf = KK[dr][t % 2], LF[dr][t % 2]
                cx.op("act", lambda e, kk=kk, lf=lf: e.activation(out=lf.t[:], in_=kk.t[:], func=ACT.Ln, scale=-1.0, bias=1.0),
                      reads=[kk.b], writes=[lf.b])
            if not is_ctx(t):
                j = t - 2
                sgr = SGR[t % 2]
                cx.op("pool", lambda e: e.tensor_tensor(out=SG.t[:, j, :], in0=sgr.t[:], in1=hgnb.t[:], op=ALU.mult),
                      reads=[sgr.b, hgnb.b], writes=[SG_b[j]])

        def s5a(t):
            full = not is_ctx(t)
            for dr in range(2):
                kk, lf = KK[dr][t % 2], LF[dr][t % 2]

                def mm1(e, dr=dr, lf=lf, kk=kk):
                    e.matmul(pbT[dr], lhsT=lf.t[:], rhs=self.tri[dr], start=True, stop=True)
                    last = e.matmul(prem[dr], lhsT=self.triS[dr], rhs=lf.t[:], start=True, stop=True)
                    if full:
                        last = e.transpose(pkT[dr], kk.t[:], self.identF)
                    return last
                cx.op("pe", mm1, reads=[lf.b, kk.b], writes=[BD[dr]])
            for dr in range(2):
                eb, er = EB[dr][t % 3], ER[dr][t % 2]
                cx.op("act", lambda e, dr=dr, eb=eb: e.activation(out=eb.t[:], in_=pbT[dr], func=ACT.Exp), reads=[BD[dr]], writes=[eb.b])
                cx.op("act", lambda e, dr=dr, er=er: e.activation(out=er.t[:], in_=prem[dr], func=ACT.Exp), reads=[BD[dr]], writes=[er.b])
            if full:
                for dr in range(2):
                    nr = NR[dr][t % 2]
                    cx.op("dve", lambda e, dr=dr, nr=nr: e.tensor_scalar(out=nr.t[:, 0:1], in0=pbT[dr][:, 64:65], scalar1=-1.0, scalar2=None, op0=ALU.mult),
                          reads=[BD[dr]], writes=[nr.b])
                    cx.op("dve", lambda e, dr=dr, nr=nr: e.tensor_copy(out=nr.t[:, 1:2], in_=pbT[dr][:, 64:65]), reads=[BD[dr]], writes=[nr.b])

        def s5b(t):
            full = not is_ctx(t)
            j = t - 2
            if full:
                for dr in range(2):
                    nr, aa, bi = NR[dr][t % 2], AA[dr][t % 2], BI[dr][t % 2]
                    cx.op("act", lambda e, dr=dr, aa=aa, nr=nr: e.activation(out=aa.t[:], in_=pbT[dr], func=ACT.Exp, bias=nr.t[:, 0:1]),
                          reads=[BD[dr], nr.b], writes=[aa.b])
                    cx.op("act", lambda e, dr=dr, bi=bi, nr=nr: e.activation(out=bi.t[:], in_=pbT[dr], func=ACT.Exp, scale=-1.0, bias=nr.t[:, 1:2]),
                          reads=[BD[dr], nr.b], writes=[bi.b])
            for dr in range(2):
                kk, er, kp = KK[dr][t % 2], ER[dr][t % 2], KP[dr][t % 2]
                cx.op("pool", lambda e, kp=kp, kk=kk, er=er: e.tensor_tensor(out=kp.t[:], in0=kk.t[:], in1=er.t[:], op=ALU.mult),
                      reads=[kk.b, er.b], writes=[kp.b])
            if full:
                qt = QT[t % 3]
                eb0, eb1 = EB[0][t % 3], EB[1][t % 3]
                qpf = QPF[t % 3]
                cx.op("dve", lambda e: e.tensor_tensor(out=qpf.t[:], in0=qt.t[:], in1=eb0.t[:], op=ALU.mult),
                      reads=[qt.b, eb0.b], writes=[qpf.b])
                cx.op("dve", lambda e: e.tensor_tensor(out=QB.t[:, j, :], in0=qt.t[:], in1=eb1.t[:], op=ALU.mult),
                      reads=[qt.b, eb1.b], writes=[QB_b[j]])
                for dr in range(2):
                    bi, kpp = BI[dr][t % 2], KPP[dr][t % 2]
                    cx.op("dve", lambda e, dr=dr, kpp=kpp, bi=bi: e.tensor_tensor(out=kpp.t[:], in0=pkT[dr], in1=bi.t[:], op=ALU.mult),
                          reads=[BD[dr], bi.b], writes=[kpp.b])
                for dr in range(2):
                    aa, qpp = AA[dr][t % 2], QPP[dr][t % 2]
                    cx.op("pool", lambda e, qpp=qpp, aa=aa: e.tensor_tensor(out=qpp.t[:], in0=qt.t[:], in1=aa.t[:], op=ALU.mult),
                          reads=[qt.b, aa.b], writes=[qpp.b])

        def s6(t):
            full = not is_ctx(t)
            vt = VT[t % 5]
            slot = (t - 2) if full else (64 + t)

            def mm(e):
                last = None
                for dr in range(2):
                    if full:
                        e.matmul(psT[dr], lhsT=KPP[dr][t % 2].t[:], rhs=QPP[dr][t % 2].t[:], start=True, stop=True)
                for dr in range(2):
                    last = e.matmul(pKV[dr], lhsT=KP[dr][t % 2].t[:], rhs=vt.t[:], start=True, stop=True)
                return last
            rd = [KP[0][t % 2].b, KP[1][t % 2].b, vt.b] + ([KPP[0][t % 2].b, KPP[1][t % 2].b, QPP[0][t % 2].b, QPP[1][t % 2].b] if full else [])
            cx.op("pe", mm, reads=rd, writes=([BsT] if full else []) + [BKV])
            if full:
                for dr in range(2):
                    stm = STM[dr][t % 2]
                    cx.op("dve", lambda e, dr=dr, stm=stm: e.copy_predicated(out=stm.t[:], mask=MSK.t[:, dr * 128:(dr + 1) * 128], data=psT[dr]),
                          reads=[BsT, MSK.b], writes=[stm.b])
            cx.op("act", lambda e: e.copy(out=KVB.t[:, slot, :], in_=pKV[1]), reads=[BKV], writes=[KVB_b[slot]])
            eb1 = EB[1][t % 3]
            cx.op("pool", lambda e: e.tensor_copy(out=ETB.t[:, slot:slot + 1], in_=eb1.t[:, 0:1]), reads=[eb1.b], writes=[ETB_b[slot]])

        def s7(t):
            full = not is_ctx(t)
            if full:
                j = t - 2
                vt = VT[t % 5]; qpf = QPF[t % 3]

                def mmo(e):
                    e.matmul(po, lhsT=STM[0][t % 2].t[:], rhs=vt.t[:], start=True, stop=False)
                    e.matmul(po, lhsT=STM[1][t % 2].t[:], rhs=vt.t[:], start=False, stop=False)
                    return e.matmul(po, lhsT=qpf.t[:], rhs=Sfb.t[:], start=False, stop=True)
                cx.op("pe", mmo, reads=[STM[0][t % 2].b, STM[1][t % 2].b, vt.b, qpf.b, Sfb.b], writes=[Bo])
            eb0 = EB[0][t % 3]
            cx.op("dve", lambda e: e.scalar_tensor_tensor(out=Sf.t[:], in0=Sf.t[:], scalar=eb0.t[:, 127:128], in1=pKV[0],
                                                          op0=ALU.mult, op1=ALU.add), reads=[Sf.b, eb0.b, BKV], writes=[Sf.b])
            cx.op("dve", lambda e: e.tensor_copy(out=Sfb.t[:], in_=Sf.t[:]), reads=[Sf.b], writes=[Sfb.b])
            if full:
                cx.op("act", lambda e: e.copy(out=OACC.t[:, j, :], in_=po), reads=[Bo], writes=[OACC_b[j]])
            st8.pop(t, None)

        stages = [(8, s7), (7, s6), (6, s5b), (5, s5a), (4, s4), (3, s3), (2, s2), (1, s1), (0, s0)]
        for i in range(NTT + 8):
            for off, fn in stages:
                t = i - off
                if 0 <= t < NTT:
                    fn(t)

        def sb_update(slot):
            cx.op("dve", lambda e: e.scalar_tensor_tensor(out=Sb.t[:], in0=Sb.t[:], scalar=ETB.t[:, slot:slot + 1], in1=KVB.t[:, slot, :],
                                                          op0=ALU.mult, op1=ALU.add),
                  reads=[Sb.b, ETB_b[slot], KVB_b[slot]], writes=[Sb.b])
            cx.op("dve", lambda e: e.tensor_copy(out=Sbb.t[:], in_=Sb.t[:]), reads=[Sb.b], writes=[Sbb.b])
        sb_update(65)
        sb_update(64)
        if "state" in self.debug:
            cx.dma(self.dbg("s_f", [128, 128]), Sf.t[:], reads=[Sf.b])
            cx.dma(self.dbg("s_b", [128, 128]), Sb.t[:], reads=[Sb.b])
        OT = self.sbn(st, "ot", [128, 128], F32, 3); YA = self.sbn(st, "ya", [128, 128], BF16, 2)
        RS = self.sbn(st, "rs", [128, 4], F32, 3); JK2 = self.sb(st, "jk2", [128, 128], BF16)
        order = list(range(NTILE - 1, -1, -1))

        def r1(idx):
            j = order[idx]; ot = OT[idx % 3]
            cx.op("pe", lambda e: e.matmul(po2, lhsT=QB.t[:, j, :], rhs=Sbb.t[:], start=True, stop=True),
                  reads=[QB_b[j], Sbb.b], writes=[Bo2])
            cx.op("dve", lambda e: e.tensor_tensor(out=ot.t[:], in0=po2, in1=OACC.t[:, j, :], op=ALU.add),
                  reads=[Bo2, OACC_b[j]], writes=[ot.b])
            sb_update(j)

        def r2(idx):
            j = order[idx]; ot = OT[idx % 3]; rs = RS[idx % 3]; ya = YA[idx % 2]
            if "ot" in self.debug:
                if "ot" not in self.dbg_out:
                    self.dbg("ot", [128, 64 * 128]); self.dbg("rs", [128, 64 * 4])
                cx.dma(self.dbg_out["ot"].ap()[:, j * 128:(j + 1) * 128], ot.t[:], reads=[ot.b])
            cx.op("act", lambda e: e.activation(out=JK2.t[:], in_=ot.t[:], func=ACT.Square, accum_out=rs.t[:, 0:1]),
                  reads=[ot.b], writes=[rs.b, JK2.b])
            cx.op("act", lambda e: e.activation(out=rs.t[:, 1:2], in_=rs.t[:, 0:1], func=ACT.Sqrt, scale=1.0 / 128, bias=EPS),
                  reads=[rs.b], writes=[rs.b])
            cx.op("dve", lambda e: e.reciprocal(out=rs.t[:, 2:3], in_=rs.t[:, 1:2]), reads=[rs.b], writes=[rs.b])
            cx.op("dve", lambda e: e.scalar_tensor_tensor(out=ya.t[:], in0=ot.t[:], scalar=rs.t[:, 2:3], in1=SG.t[:, j, :],
                                                          op0=ALU.mult, op1=ALU.mult),
                  reads=[ot.b, rs.b, SG_b[j]], writes=[ya.b])
            if "ot" in self.debug:
                cx.dma(self.dbg_out["rs"].ap()[:, j * 4:(j + 1) * 4], rs.t[:], reads=[rs.b])

        def r3(idx):
            j = order[idx]; ya = YA[idx % 2]
            pv = self.psb(0)
            cx.op("pe", lambda e: e.transpose(pv[:, 0:128], ya.t[:], self.identB), reads=[ya.b], writes=[self.PS[0].b])
            yg = YAT[(j // 8) % 2]
            cx.op("act", lambda e: e.copy(out=yg.t[:, (j % 8) * 128:(j % 8 + 1) * 128], in_=pv[:, 0:128]),
                  reads=[self.PS[0].b], writes=[yg.b])
            if j % 8 == 0:
                cx.dma(self.ag_in.ap()[0:128, j * 128:(j + 8) * 128], yg.t[:], reads=[yg.b], writes=[self.B_ag_in])
                if "yat" in self.debug:
                    if "yat" not in self.dbg_out:
                        self.dbg("yat", [128, L], BF16)
                    cx.dma(self.dbg_out["yat"].ap()[:, j * 128:(j + 8) * 128], yg.t[:], reads=[yg.b])

        for i in range(NTILE + 2):
            for off, fn in ((2, r3), (1, r2), (0, r1)):
                idx = i - off
                if 0 <= idx < NTILE:
                    fn(idx)

    def phase_1c(self, st):
        cx = self.cx
        NX = 127 * 128
        TS = self.sbn(st, "tsk", [128, NX], BF16, 2)
        YT = self.sb(st, "ytok", [128, 128, 64], BF16)
        YT_b = [Buf() for _ in range(16)]
        YBT = self.sb(st, "ybt", [64, NB * L], BF16)
        ge = self.gext
        for c in range(64):
            ts = TS[c % 2]
            src = bass.AP(ge, c * 2 * L + 1, [[1, 128], [1, NX]])
            for q4 in range(4):
                x0, x1 = q4 * (NX // 4), (q4 + 1) * (NX // 4)
                srcq = bass.AP(ge, c * 2 * L + 1 + x0, [[1, 128], [1, x1 - x0]])
                cx.dma(ts.t[:, x0:x1], srcq, reads=[self.B_gext], writes=[ts.b], q=("sp" if q4 % 2 == 0 else "pool"))
            bank = (c // 4) % 8
            py = self.PS[bank].t[:, (c % 4) * 128:(c % 4 + 1) * 128]
            py3 = py.rearrange("p (b i) -> p b i", b=2)
            wc = self.Wp.t[:, c, :]
            wc3 = wc.rearrange("p (b j) -> p b j", b=2)

            def mm(e, ts=ts, py=py, py3=py3, wc=wc, wc3=wc3):
                e.matmul(py, lhsT=ts.t[:, 63 * 128:64 * 128], rhs=wc, start=True, stop=False)
                last = None
                ds_ = [d for d in range(-63, 64) if d != 0]
                for idx, d in enumerate(ds_):
                    j0, j1 = max(0, -d), min(64, 64 - d)
                    last = e.matmul(py3[:, :, j0 + d:j1 + d], lhsT=ts.t[:, (d + 63) * 128:(d + 64) * 128], rhs=wc3[:, :, j0:j1],
                                    start=False, stop=(idx == len(ds_) - 1))
                return last
            cx.op("pe", mm, reads=[ts.b] + self.Wp_b, writes=[self.PS[bank].b])
            cx.op("dve", lambda e, c=c, py=py: e.tensor_tensor(out=YT.t[:, :, c], in0=py, in1=self.X0.t[:, c, :], op=ALU.mult),
                  reads=[self.PS[bank].b] + (self.X0_b if c == 0 else []), writes=[YT_b[c // 4]])
        for g in range(16):
            bank = g % 2
            pv = self.psb(bank)

            def tr(e, g=g, pv=pv):
                last = None
                for i in range(8):
                    n = g * 8 + i
                    last = e.transpose(pv[0:64, i * 128:(i + 1) * 128], YT.t[:, n, :], self.identB)
                return last
            cx.op("pe", tr, reads=YT_b, writes=[self.PS[bank].b])
            cx.op("act" if g % 2 == 0 else "dve",
                  (lambda e, g=g, pv=pv: e.copy(out=YBT.t[:, g * 1024:(g + 1) * 1024], in_=pv[0:64, :])) if g % 2 == 0 else
                  (lambda e, g=g, pv=pv: e.tensor_copy(out=YBT.t[:, g * 1024:(g + 1) * 1024], in_=pv[0:64, :])),
                  reads=[self.PS[bank].b], writes=[YBT.b])
        dst = self.ag_in.ap()[128:256, :].rearrange("(c h) t -> c h t", h=2)
        cx.dma(dst, YBT.t[:].rearrange("c (h t) -> c h t", h=2), reads=[YBT.b], writes=[self.B_ag_in])
        if "ybt" in self.debug:
            cx.dma(self.dbg("ybt", [64, NB * L], BF16), YBT.t[:], reads=[YBT.b])

    def phase_exchange(self):
        cx, nc = self.cx, self.nc
        if "noag" in self.debug:
            return

        def emit(e, sem):
            e.collective_compute("AllGather", ALU.bypass, replica_groups=[list(range(NCORES))],
                                 ins=[self.ag_in.ap()], outs=[self.ag_out.ap()]).then_inc(sem, 1)
        cx.custom("pool", emit, reads=[self.B_ag_in], writes=[self.B_ag_out])

    def phase_2(self, st):
        cx, nc = self.cx, self.nc
        R = "right"
        sbr = lambda stt, name, shape, dtype=F32: TB(stt.enter_context(nc.sbuf_tensor("sr_" + name, list(shape), dtype, side=R)), name)
        gt1b = self.gtb.t[:, 0:1024]; gt2b = self.gtb.t[:, 1024:2048]
        pid = nc.sync.partition_id()
        bq = pid // 4
        qq = pid % 4
        with contextlib.ExitStack() as mst:
            MIX = self.sb(mst, "mixT", [128, 8, TOK2], BF16)
            MIX_b = [[Buf() for _ in range(4)] for _ in range(8)]
            with contextlib.ExitStack() as hst:
                HXM = self.sb(hst, "hxm", [128, 8, TOK2], BF16)
                HXM_b = [Buf() for _ in range(16)]
                with contextlib.ExitStack() as a0:
                    self.alloc_norm(a0, nslot=2)
                    stn = {}

                    def n0(tt):
                        stn[tt] = self.norm_load(self.x_mine[tt * 128:(tt + 1) * 128, :],
                                                 (self.posr_mine[2 * tt:2 * tt + 1, :], self.posr_mine[2 * tt + 1:2 * tt + 2, :]))

                    def n1(tt):
                        self.norm_stats(stn[tt])

                    def n2(tt):
                        v = TB(_HXView(HXM.t, tt)); v.b = HXM_b[tt]
                        self.norm_tr(stn[tt], 3, v, ps_bank=(tt % 2))
                    for i in range(16 + 2):
                        for off, fn in ((2, n2), (1, n1), (0, n0)):
                            if 0 <= i - off < 16:
                                fn(i - off)
                    cx.barrier()
                with contextlib.ExitStack() as a1:
                    YA2 = self.sb(a1, "ya2", [128, 4, TOK2], BF16); YB2 = self.sb(a1, "yb2", [128, 4, TOK2], BF16)
                    WGT = self.sbn(a1, "wgt", [128, 8, 256], BF16, 2)
                    WPA = self.sb(a1, "wpa", [128, 4, D], BF16); WPB = self.sb(a1, "wpb", [128, 4, D], BF16)
                    SA = self.sbn(a1, "sga", [128, 512], F32, 2); SB_ = self.sbn(a1, "sgb", [128, 512], F32, 2)
                    M1 = self.sbn(a1, "m1", [128, 512], F32, 2); M2 = self.sbn(a1, "m2", [128, 512], F32, 2)
                    if "noag" in self.debug:
                        ya_src = self.din("ya_in", [512, TOK2], BF16); yb_src = self.din("yb_in", [512, TOK2], BF16)
                        cx.dma(YA2.t[:], ya_src.rearrange("(k p) t -> p k t", p=128), writes=[YA2.b])
                        cx.dma(YB2.t[:], yb_src.rearrange("(k p) t -> p k t", p=128), writes=[YB2.b])
                    else:
                        ago = self.ag_out.ap()
                        va = ago.rearrange("(b h r) (q t) -> b q h r t", b=2, h=4, r=256, q=4)
                        srca = va[bass.ds(bq, 1), bass.ds(qq, 1), :, 0:128, :].rearrange("b q h p t -> p (b q h) t")
                        cx.dma(YA2.t[:], srca, reads=[self.B_ag_out], writes=[YA2.b])
                        vb = ago.rearrange("(k two c h) (q t) -> two h q k c t", k=4, two=2, c=128, h=2, q=4)
                        for par in range(2):
                            srcb = vb[par, bass.ds(bq, 1), bass.ds(qq, 1), :, 64:128, :].rearrange("h q k c t -> c (h q k) t")
                            cx.dma(YB2.t[par * 64:(par + 1) * 64, :, :], srcb, reads=[self.B_ag_out], writes=[YB2.b])
                    if "ya2" in self.debug:
                        cx.dma(self.dbg("ya2", [128, 4 * TOK2], BF16), YA2.t[:].rearrange("p k t -> p (k t)"), reads=[YA2.b])
                        cx.dma(self.dbg("yb2", [128, 4 * TOK2], BF16), YB2.t[:].rearrange("p k t -> p (k t)"), reads=[YB2.b])
                    cx.dma(WPA.t[:], self.w_pa.rearrange("(k p) c -> p k c", p=128), writes=[WPA.b], q="pool")
                    cx.dma(WPB.t[:], self.w_pb.rearrange("(k p) c -> p k c", p=128), writes=[WPB.b], q="pool")
                    wg_v = self.w_gate.rearrange("(k p) c -> p k c", p=128)
                    it = 0
                    def wg_load(c):
                        wg_ = WGT[c % 2]
                        cx.dma(wg_.t[:, :, 0:128], wg_v[:, :, c * 128:(c + 1) * 128], writes=[wg_.b], q="pool")
                        cx.dma(wg_.t[:, :, 128:256], wg_v[:, :, 1024 + c * 128:1024 + (c + 1) * 128], writes=[wg_.b], q="pool")
                    wg_load(0)
                    for c in range(8):
                        wg = WGT[c % 2]
                        if c + 1 < 8:
                            wg_load(c + 1)
                        for tb in range(4):
                            s = it % 2; it += 1
                            bs = 4 * s
                            pga, pgb, ppa, ppb = self.PS[bs], self.PS[bs + 1], self.PS[bs + 2], self.PS[bs + 3]
                            tsl = slice(tb * 512, (tb + 1) * 512)

                            def mm(e, wg=wg, c=c, tsl=tsl, pga=pga, pgb=pgb, ppa=ppa, ppb=ppb):
                                for k in range(8):
                                    e.matmul(pga.t[:, :], lhsT=wg.t[:, k, 0:128], rhs=HXM.t[:, k, tsl], start=(k == 0), stop=(k == 7))
                                for k in range(8):
                                    e.matmul(pgb.t[:, :], lhsT=wg.t[:, k, 128:256], rhs=HXM.t[:, k, tsl], start=(k == 0), stop=(k == 7))
                                for k in range(4):
                                    e.matmul(ppa.t[:, :], lhsT=WPA.t[:, k, c * 128:(c + 1) * 128], rhs=YA2.t[:, k, tsl], start=(k == 0), stop=(k == 3))
                                last = None
                                for k in range(4):
                                    last = e.matmul(ppb.t[:, :], lhsT=WPB.t[:, k, c * 128:(c + 1) * 128], rhs=YB2.t[:, k, tsl],
                                                    start=(k == 0), stop=(k == 3))
                                return last
                            cx.op("pe", mm, reads=[wg.b, WPA.b, WPB.b, YA2.b, YB2.b] + HXM_b[tb * 4:(tb + 1) * 4],
                                  writes=[pga.b, pgb.b, ppa.b, ppb.b])
                            cx.op("act", lambda e, s=s, pga=pga: e.activation(out=SA[s].t[:], in_=pga.t[:, :], func=ACT.Sigmoid),
                                  reads=[pga.b], writes=[SA[s].b])
                            cx.op("act", lambda e, s=s, pgb=pgb: e.activation(out=SB_[s].t[:], in_=pgb.t[:, :], func=ACT.Sigmoid),
                                  reads=[pgb.b], writes=[SB_[s].b])
                            cx.op("dve", lambda e, s=s, ppa=ppa: e.tensor_tensor(out=M1[s].t[:], in0=ppa.t[:, :], in1=SA[s].t[:], op=ALU.mult),
                                  reads=[ppa.b, SA[s].b], writes=[M1[s].b])
                            cx.op("dve", lambda e, s=s, ppb=ppb: e.tensor_tensor(out=M2[s].t[:], in0=ppb.t[:, :], in1=SB_[s].t[:], op=ALU.mult),
                                  reads=[ppb.b, SB_[s].b], writes=[M2[s].b])
                            cx.op("pool", lambda e, s=s, c=c, tsl=tsl: e.tensor_tensor(out=MIX.t[:, c, tsl], in0=M1[s].t[:], in1=M2[s].t[:], op=ALU.add),
                                  reads=[M1[s].b, M2[s].b], writes=[MIX_b[c][tb]])
                    cx.barrier()
            cx.barrier()
            rst = contextlib.ExitStack()
            self.st.enter_context(rst)
            H2T = sbr(rst, "h2t", [128, 8, TOK2], BF16); H2T_b = [Buf() for _ in range(16)]
            LG = sbr(rst, "lg", [128, 16, 36]); LG_b = Buf()
            COMB = sbr(rst, "comb", [128, 16, 32])
            with contextlib.ExitStack() as a2:
                self.alloc_norm(a2, nslot=2)
                WO = self.sb(a2, "wo", [128, 8, D], BF16); WR = self.sb(a2, "wr", [128, 8, 36]); brb = self.sb(a2, "brb", [128, 36])
                cx.dma(WO.t[:], self.w_out.rearrange("(k p) c -> p k c", p=128), writes=[WO.b], q="pool")
                cx.dma(WR.t[:], self.wr.rearrange("(k p) c -> p k c", p=128), writes=[WR.b])
                cx.dma(brb.t[:], self.br.partition_broadcast(128), writes=[brb.b])
                X1 = self.sbn(a2, "x1t", [128, D], F32, 2); TMP = self.sbn(a2, "tmpo", [128, 512], F32, 2)
                XN32 = self.sbn(a2, "xn32", [128, D], F32, 2); HT32 = self.sbn(a2, "ht32", [128, 8, 128], F32, 2)
                def sa(tt):
                    s = tt % 2
                    x1 = X1[s]
                    i = self.norm_i; self.norm_i += 1
                    xt = self.XT[i % len(self.XT)]; pt = self.PT[i % len(self.PT)]
                    cx.dma(xt.t[:], self.x_mine[tt * 128:(tt + 1) * 128, :], writes=[xt.b])
                    cx.dma(pt.t[0:64, :], self.posr_mine[2 * tt:2 * tt + 1, :].partition_broadcast(64), writes=[pt.b])
                    cx.dma(pt.t[64:128, :], self.posr_mine[2 * tt + 1:2 * tt + 2, :].partition_broadcast(64), writes=[pt.b])
                    cx.op("pool", lambda e, xt=xt, pt=pt: e.tensor_tensor(out=xt.t[:, 0:512], in0=xt.t[:, 0:512], in1=pt.t[:], op=ALU.add),
                          reads=[xt.b, pt.b], writes=[xt.b])
                    cx.op("pool", lambda e, xt=xt: e.tensor_tensor(out=xt.t[:, 512:1024], in0=xt.t[:, 512:1024], in1=self.posc.t[:], op=ALU.add),
                          reads=[xt.b], writes=[xt.b])
                    for half in range(2):
                        po = self.PS[2 + half]
                        hs = slice(half * 512, (half + 1) * 512)

                        def mmo(e, tt=tt, hs=hs, po=po):
                            last = None
                            for k in range(8):
                                last = e.matmul(po.t[:, :], lhsT=MIX.t[:, k, tt * 128:(tt + 1) * 128], rhs=WO.t[:, k, hs],
                                                start=(k == 0), stop=(k == 7))
                            return last
                        cx.op("pe", mmo, reads=[WO.b] + [MIX_b[k][tt // 4] for k in range(8)], writes=[po.b])
                        tm = TMP[half]
                        cx.op("dve", lambda e, tm=tm, po=po, hs=hs: e.tensor_tensor(out=tm.t[:], in0=po.t[:, :], in1=gt1b[:, hs], op=ALU.mult),
                              reads=[po.b, self.gtb.b], writes=[tm.b])
                        cx.op("pool", lambda e, tm=tm, x1=x1, xt=xt, hs=hs: e.tensor_tensor(out=x1.t[:, hs], in0=tm.t[:], in1=xt.t[:, hs], op=ALU.add),
                              reads=[tm.b, xt.b], writes=[x1.b])
                    cx.dma(self.x1_scr.ap()[tt * 128:(tt + 1) * 128, :], x1.t[:], reads=[x1.b], writes=[self.B_x1_scr[tt]])
                    if "x1" in self.debug:
                        if tt == 0:
                            self.dbg("x1", [TOK2, D])
                        cx.dma(self.dbg_out["x1"].ap()[tt * 128:(tt + 1) * 128, :], x1.t[:], reads=[x1.b])

                def sb2(tt):
                    s = tt % 2
                    x1 = X1[s]
                    ss = self.SS[tt % 4]; xn = XN32[s]
                    cx.op("act", lambda e, x1=x1, ss=ss: e.activation(out=self.JK.t[:], in_=x1.t[:], func=ACT.Square, accum_out=ss.t[:, 0:1]),
                          reads=[x1.b], writes=[ss.b, self.JK.b])
                    cx.op("act", lambda e, ss=ss: e.activation(out=ss.t[:, 1:2], in_=ss.t[:, 0:1], func=ACT.Sqrt, scale=1.0 / D, bias=EPS),
                          reads=[ss.b], writes=[ss.b])
                    cx.op("dve", lambda e, ss=ss: e.reciprocal(out=ss.t[:, 2:3], in_=ss.t[:, 1:2]), reads=[ss.b], writes=[ss.b])
                    cx.op("dve", lambda e, x1=x1, xn=xn, ss=ss: e.tensor_scalar(out=xn.t[:], in0=x1.t[:], scalar1=ss.t[:, 2:3], scalar2=None, op0=ALU.mult),
                          reads=[x1.b, ss.b], writes=[xn.b])

                def sc(tt):
                    s = tt % 2
                    xn = XN32[s]; ht = HT32[s]
                    for hf in range(2):
                        pb = self.PS[4 + hf]

                        def tr(e, xn=xn, pb=pb, hf=hf):
                            last = None
                            for k4 in range(4):
                                k = hf * 4 + k4
                                last = e.transpose(pb.t[:, k4 * 128:(k4 + 1) * 128], xn.t[:, k * 128:(k + 1) * 128], self.identF)
                            return last
                        cx.op("pe", tr, reads=[xn.b], writes=[pb.b])
                        for k4 in range(4):
                            k = hf * 4 + k4
                            cx.op("act", lambda e, k=k, k4=k4, pb=pb, ht=ht: e.activation(out=ht.t[:, k, :], in_=pb.t[:, k4 * 128:(k4 + 1) * 128],
                                                                                     func=ACT.Identity, scale=self.gsc2.t[:, k:k + 1], bias=self.sh2(k)),
                                  reads=[pb.b, self.gsc2.b], writes=[ht.b])
                    cx.op("pool", lambda e, tt=tt, ht=ht: e.tensor_copy(out=H2T.t[:, :, tt * 128:(tt + 1) * 128], in_=ht.t[:]),
                          reads=[ht.b], writes=[H2T_b[tt]])
                    pl = self.PS[6 + (tt % 2)]

                    def mml(e, ht=ht, pl=pl):
                        last = None
                        for k in range(8):
                            last = e.matmul(pl.t[:, 0:36], lhsT=ht.t[:, k, :], rhs=WR.t[:, k, :], start=(k == 0), stop=(k == 7))
                        return last
                    cx.op("pe", mml, reads=[ht.b, WR.b], writes=[pl.b])
                    cx.op("dve", lambda e, tt=tt, pl=pl: e.tensor_tensor(out=LG.t[:, tt, :], in0=pl.t[:, 0:36], in1=brb.t[:], op=ALU.add),
                          reads=[pl.b, brb.b], writes=[LG_b])

                for i in range(16 + 2):
                    for off, fn in ((2, sc), (1, sb2), (0, sa)):
                        if 0 <= i - off < 16:
                            fn(i - off)
                cx.barrier()
        cx.barrier()
        self.route(rst, LG, LG_b, COMB)
        self.moe_and_out(rst, sbr, H2T, H2T_b, COMB, gt2b)


class _HXView:
    def __init__(self, t, tt):
        self.t = t
        self.tt = tt

    def __getitem__(self, key):
        p, k, f = key
        assert f == slice(None)
        return self.t[p, k, self.tt * 128:(self.tt + 1) * 128]


def _route(self, st, LG, LG_b, COMB):
    cx = self.cx
    with contextlib.ExitStack() as ts:
        def T(name, w):
            return self.sb(ts, "rt_" + name, [128, 16, w])
        gmax = T("gmax", 1); ohg = T("ohg", 4); exg = T("exg", 4); sumg = T("sumg", 1); ptg = T("ptg", 1)
        esel = T("esel", 8); tmp8 = T("tmp8", 8); m1 = T("m1", 1); mask1 = T("mask1", 8); e2 = T("e2", 8)
        m2 = T("m2", 1); mask2 = T("mask2", 8); dlt = T("dlt", 1); w1 = T("w1", 1); w2 = T("w2", 1); cl = T("cl", 8)
        lgG = LG.t[:, :, 0:4]
        def bc(x, w):
            return x.t[:, :, 0:1].to_broadcast([128, 16, w])

        def dve(fn, reads, writes):
            cx.op("dve", fn, reads=reads, writes=writes)
        dve(lambda e: e.tensor_reduce(out=gmax.t[:, :, 0], in_=lgG, axis=AX.X, op=ALU.max), [LG_b], [gmax.b])
        dve(lambda e: e.tensor_tensor(out=ohg.t[:], in0=lgG, in1=bc(gmax, 4), op=ALU.is_equal), [LG_b, gmax.b], [ohg.b])
        dve(lambda e: e.tensor_tensor(out=exg.t[:], in0=lgG, in1=bc(gmax, 4), op=ALU.subtract), [LG_b, gmax.b], [exg.b])
        cx.op("act", lambda e: e.activation(out=exg.t[:], in_=exg.t[:], func=ACT.Exp), reads=[exg.b], writes=[exg.b])
        dve(lambda e: e.tensor_reduce(out=sumg.t[:, :, 0], in_=exg.t[:], axis=AX.X, op=ALU.add), [exg.b], [sumg.b])
        dve(lambda e: e.reciprocal(out=ptg.t[:], in_=sumg.t[:]), [sumg.b], [ptg.b])
        for g in range(4):
            src = LG.t[:, :, 4 + 8 * g:12 + 8 * g]
            ohb = ohg.t[:, :, g:g + 1].to_broadcast([128, 16, 8])
            if g == 0:
                dve(lambda e, src=src, ohb=ohb: e.tensor_tensor(out=esel.t[:], in0=src, in1=ohb, op=ALU.mult), [LG_b, ohg.b], [esel.b])
            else:
                dve(lambda e, src=src, ohb=ohb: e.tensor_tensor(out=tmp8.t[:], in0=src, in1=ohb, op=ALU.mult), [LG_b, ohg.b], [tmp8.b])
                dve(lambda e: e.tensor_tensor(out=esel.t[:], in0=esel.t[:], in1=tmp8.t[:], op=ALU.add), [esel.b, tmp8.b], [esel.b])
        dve(lambda e: e.tensor_reduce(out=m1.t[:, :, 0], in_=esel.t[:], axis=AX.X, op=ALU.max), [esel.b], [m1.b])
        dve(lambda e: e.tensor_tensor(out=mask1.t[:], in0=esel.t[:], in1=bc(m1, 8), op=ALU.is_equal), [esel.b, m1.b], [mask1.b])
        dve(lambda e: e.scalar_tensor_tensor(out=e2.t[:], in0=mask1.t[:], scalar=-1e30, in1=esel.t[:], op0=ALU.mult, op1=ALU.add),
            [mask1.b, esel.b], [e2.b])
        dve(lambda e: e.tensor_reduce(out=m2.t[:, :, 0], in_=e2.t[:], axis=AX.X, op=ALU.max), [e2.b], [m2.b])
        dve(lambda e: e.tensor_tensor(out=mask2.t[:], in0=e2.t[:], in1=bc(m2, 8), op=ALU.is_equal), [e2.b, m2.b], [mask2.b])
        dve(lambda e: e.tensor_tensor(out=dlt.t[:], in0=m2.t[:], in1=m1.t[:], op=ALU.subtract), [m1.b, m2.b], [dlt.b])
        cx.op("act", lambda e: e.activation(out=dlt.t[:], in_=dlt.t[:], func=ACT.Exp), reads=[dlt.b], writes=[dlt.b])
        dve(lambda e: e.tensor_scalar(out=w1.t[:], in0=dlt.t[:], scalar1=1.0, scalar2=None, op0=ALU.add), [dlt.b], [w1.b])
        dve(lambda e: e.reciprocal(out=w1.t[:], in_=w1.t[:]), [w1.b], [w1.b])
        dve(lambda e: e.tensor_tensor(out=w2.t[:], in0=dlt.t[:], in1=w1.t[:], op=ALU.mult), [dlt.b, w1.b], [w2.b])
        dve(lambda e: e.tensor_tensor(out=w1.t[:], in0=w1.t[:], in1=ptg.t[:], op=ALU.mult), [w1.b, ptg.b], [w1.b])
        dve(lambda e: e.tensor_tensor(out=w2.t[:], in0=w2.t[:], in1=ptg.t[:], op=ALU.mult), [w2.b, ptg.b], [w2.b])
        dve(lambda e: e.tensor_tensor(out=cl.t[:], in0=mask1.t[:], in1=bc(w1, 8), op=ALU.mult), [mask1.b, w1.b], [cl.b])
        dve(lambda e: e.tensor_tensor(out=tmp8.t[:], in0=mask2.t[:], in1=bc(w2, 8), op=ALU.mult), [mask2.b, w2.b], [tmp8.b])
        dve(lambda e: e.tensor_tensor(out=cl.t[:], in0=cl.t[:], in1=tmp8.t[:], op=ALU.add), [cl.b, tmp8.b], [cl.b])
        for g in range(4):
            ohb = ohg.t[:, :, g:g + 1].to_broadcast([128, 16, 8])
            dve(lambda e, g=g, ohb=ohb: e.tensor_tensor(out=COMB.t[:, :, 8 * g:8 * g + 8], in0=cl.t[:], in1=ohb, op=ALU.mult),
                [cl.b, ohg.b], [COMB.b])
        if "comb" in self.debug:
            cx.dma(self.dbg("comb", [128, 16 * 32]), COMB.t[:].rearrange("p a c -> p (a c)"), reads=[COMB.b])
            cx.dma(self.dbg("lg", [128, 16 * 36]), LG.t[:].rearrange("p a c -> p (a c)"), reads=[LG_b])
        cx.barrier()


def _moe_and_out(self, st, sbr, H2T, H2T_b, COMB, gt2b):
    cx = self.cx
    NE = 32 if "moe_ne" not in self.__dict__ else self.moe_ne
    ACC = sbr(st, "acc", [128, 16, D]); ACC_b = [Buf() for _ in range(16)]
    with contextlib.ExitStack() as ms:
        WG = self.sbn(ms, "mwg", [128, 8, 512], BF16, 2); WU = self.sbn(ms, "mwu", [128, 8, 512], BF16, 2)
        WD = self.sbn(ms, "mwd", [128, 4, D], BF16, 2)
        HID = self.sb(ms, "hid", [128, 4, TOK2], BF16); HID_b = [Buf() for _ in range(4)]
        SGT = self.sbn(ms, "sgt", [128, 512], F32, 3)

        def prefetch(e):
            s = e % 2
            cx.dma(WG[s].t[:], self.moe_wg[e].rearrange("(k p) f -> p k f", p=128), writes=[WG[s].b], q="pool")
            cx.dma(WU[s].t[:], self.moe_wu[e].rearrange("(k p) f -> p k f", p=128), writes=[WU[s].b], q="pool")
            cx.dma(WD[s].t[:], self.moe_wd[e].rearrange("(k p) f -> p k f", p=128), writes=[WD[s].b], q="pool")
        prefetch(0)
        it = 0
        dn = 0
        for ex in range(NE):
            s = ex % 2
            if ex + 1 < NE:
                prefetch(ex + 1)
            wg, wu, wd = WG[s], WU[s], WD[s]
            for tb in range(4):
                tsl = slice(tb * 512, (tb + 1) * 512)
                for fc in range(4):
                    pg = self.PS[(it % 2) * 2]; pu = self.PS[(it % 2) * 2 + 1]
                    sg = SGT[it % 3]
                    it += 1

                    def mm(e, wg=wg, wu=wu, fc=fc, tsl=tsl, pg=pg, pu=pu):
                        for k in range(8):
                            e.matmul(pg.t[:, :], lhsT=wg.t[:, k, fc * 128:(fc + 1) * 128], rhs=H2T.t[:, k, tsl], start=(k == 0), stop=(k == 7))
                        last = None
                        for k in range(8):
                            last = e.matmul(pu.t[:, :], lhsT=wu.t[:, k, fc * 128:(fc + 1) * 128], rhs=H2T.t[:, k, tsl], start=(k == 0), stop=(k == 7))
                        return last
                    cx.op("pe", mm, reads=[wg.b, wu.b] + H2T_b[tb * 4:(tb + 1) * 4], writes=[pg.b, pu.b])
                    cx.op("act", lambda e, sg=sg, pg=pg: e.activation(out=sg.t[:], in_=pg.t[:, :], func=ACT.Silu), reads=[pg.b], writes=[sg.b])
                    cx.op("dve", lambda e, sg=sg, pu=pu, fc=fc, tsl=tsl: e.tensor_tensor(out=HID.t[:, fc, tsl], in0=pu.t[:, :], in1=sg.t[:], op=ALU.mult),
                          reads=[pu.b, sg.b], writes=[HID_b[tb]])
            for tt in range(16):
                for half in range(2):
                    pd = self.PS[4 + (dn % 4)]
                    dn += 1
                    hs = slice(half * 512, (half + 1) * 512)

                    def mmd(e, wd=wd, tt=tt, hs=hs, pd=pd):
                        last = None
                        for fc in range(4):
                            last = e.matmul(pd.t[:, :], lhsT=HID.t[:, fc, tt * 128:(tt + 1) * 128], rhs=wd.t[:, fc, hs], start=(fc == 0), stop=(fc == 3))
                        return last
                    cx.op("pe", mmd, reads=[wd.b, HID_b[tt // 4]], writes=[pd.b])
                    if ex == 0:
                        cx.op("dve", lambda e, tt=tt, hs=hs, pd=pd, ex=ex: e.tensor_scalar(out=ACC.t[:, tt, hs], in0=pd.t[:, :], scalar1=COMB.t[:, tt, ex:ex + 1],
                                                                                       scalar2=None, op0=ALU.mult),
                              reads=[pd.b, COMB.b], writes=[ACC_b[tt]])
                    else:
                        cx.op("dve", lambda e, tt=tt, hs=hs, pd=pd, ex=ex: e.scalar_tensor_tensor(out=ACC.t[:, tt, hs], in0=pd.t[:, :], scalar=COMB.t[:, tt, ex:ex + 1],
                                                                                              in1=ACC.t[:, tt, hs], op0=ALU.mult, op1=ALU.add),
                              reads=[pd.b, COMB.b, ACC_b[tt]], writes=[ACC_b[tt]])
        cx.barrier()
    with contextlib.ExitStack() as fs:
        X1 = self.sbn(fs, "fx1", [128, D], F32, 3); OT = self.sbn(fs, "fot", [128, D], F32, 2)
        fngb = self.sb(fs, "fngb", [128, D]); JK = self.sb(fs, "fjk", [128, D], BF16); SS = self.sbn(fs, "fss", [128, 4], F32, 4)
        cx.dma(fngb.t[:], self.fng.partition_broadcast(128), writes=[fngb.b])
        if "moe" in self.debug:
            cx.dma(self.dbg("moe", [128, 16 * D]), ACC.t[:].rearrange("p a c -> p (a c)"), reads=ACC_b)
        for tt in range(16):
            x1 = X1[tt % 3]; ot = OT[tt % 2]; ss = SS[tt % 4]
            cx.dma(x1.t[:], self.x1_scr.ap()[tt * 128:(tt + 1) * 128, :], reads=[self.B_x1_scr[tt]], writes=[x1.b])
            cx.op("dve", lambda e, tt=tt: e.tensor_tensor(out=ACC.t[:, tt, :], in0=ACC.t[:, tt, :], in1=gt2b, op=ALU.mult),
                  reads=[ACC_b[tt], self.gtb.b], writes=[ACC_b[tt]])
            cx.op("pool", lambda e, tt=tt, x1=x1: e.tensor_tensor(out=x1.t[:], in0=x1.t[:], in1=ACC.t[:, tt, :], op=ALU.add),
                  reads=[x1.b, ACC_b[tt]], writes=[x1.b])
            cx.op("act", lambda e, x1=x1, ss=ss: e.activation(out=JK.t[:], in_=x1.t[:], func=ACT.Square, accum_out=ss.t[:, 0:1]),
                  reads=[x1.b], writes=[ss.b, JK.b])
            cx.op("act", lambda e, ss=ss: e.activation(out=ss.t[:, 1:2], in_=ss.t[:, 0:1], func=ACT.Sqrt, scale=1.0 / D, bias=EPS),
                  reads=[ss.b], writes=[ss.b])
            cx.op("dve", lambda e, ss=ss: e.reciprocal(out=ss.t[:, 2:3], in_=ss.t[:, 1:2]), reads=[ss.b], writes=[ss.b])
            cx.op("dve", lambda e, x1=x1, ot=ot, ss=ss: e.scalar_tensor_tensor(out=ot.t[:], in0=x1.t[:], scalar=ss.t[:, 2:3], in1=fngb.t[:],
                                                                            op0=ALU.mult, op1=ALU.mult),
                  reads=[x1.b, ss.b, fngb.b], writes=[ot.b])
            cx.dma(self.out[tt * 128:(tt + 1) * 128, :], ot.t[:], reads=[ot.b])
        cx.barrier()


Prog.route = _route
Prog.moe_and_out = _moe_and_out


_CACHE = {}


def kernel(**inputs):
    per = prep_inputs(inputs)
    if "nc" not in _CACHE:
        p = Prog()
        _CACHE["nc"] = p.build()
        _CACHE["used"] = list(p.used_inputs)
    nc = _CACHE["nc"]
    used = set(_CACHE["used"])
    per = [{k: v for k, v in d.items() if k in used} for d in per]
    res = run_bass_kernel_spmd(nc, per, core_ids=list(range(NCORES)))
    out = np.concatenate([np.asarray(r["out"], dtype=np.float32) for r in res.results], axis=0)
    return out.reshape(NB, L, D)
```
